# Optimizing a Trainium2 kernel written in Bass

```python
import jax, jax.numpy as jnp
from jax import lax
import numpy as np

D_MODEL = 4096
BATCH = 1
SEQ = 8192
DEPTH = 1

GRID_W = 64
CTX_LEN = 256
RET_HEADS = D_MODEL // 512
RET_DK = 256
RET_DV = 256
RET_QK_W = RET_HEADS * RET_DK
RET_W = RET_HEADS * RET_DV
RET_CHUNK = 128
ROPE_BASE = 10000.0
SGU_GROUPS = D_MODEL // 512
SGU_GROUP_DIM = 256
SGU_W = SGU_GROUPS * SGU_GROUP_DIM
SGU_CHUNK = 128
MIX_W = RET_W + SGU_W
Q_OFF = 0
K_OFF = Q_OFF + RET_QK_W
V_OFF = K_OFF + RET_QK_W
G_OFF = V_OFF + RET_W
U_OFF = G_OFF + RET_W
SV_OFF = U_OFF + SGU_W
IN_W = SV_OFF + SGU_W
MOE_GROUPS = 8
EXPERTS_PER_GROUP = 8
N_EXPERTS = MOE_GROUPS * EXPERTS_PER_GROUP
MOE_TOP_K = 2
D_EXPERT = D_MODEL // 8
MOE_BLOCK = 128
N_MOD = 6
EPS = 1e-6

kernel_name = 'hybrid_retention_sgu_hmoe_block'


def rms_norm(x, g):
    x32 = x.astype(jnp.float32)
    y = x32 * lax.rsqrt(jnp.mean(x32 * x32, axis=-1, keepdims=True) + EPS)
    return (y * g.astype(jnp.float32)).astype(x.dtype)


def modulate(h, shift, scale):
    return h * (1 + scale) + shift


def split_heads(t, n_heads):
    b, l, _ = t.shape
    return t.reshape(b, l, n_heads, -1).transpose(0, 2, 1, 3)


def rope_tables(n_tokens, dtype):
    n_rows = n_tokens // GRID_W
    rows = jnp.repeat(jnp.arange(n_rows), GRID_W)
    cols = jnp.tile(jnp.arange(GRID_W), n_rows)
    n_freq = RET_DK // 4
    freqs = ROPE_BASE ** (-jnp.arange(n_freq, dtype=jnp.float32) / n_freq)
    pos = jnp.stack([rows, cols], axis=-1).astype(jnp.float32)
    ang = pos[:, :, None, None] * freqs
    ang = jnp.broadcast_to(ang, (n_tokens, 2, 2, n_freq)).reshape(n_tokens, RET_DK)
    return jnp.cos(ang).astype(dtype), jnp.sin(ang).astype(dtype)


def rope_2d(t, cos, sin):
    tr = t.reshape(t.shape[:-1] + (2, 2, RET_DK // 4))
    rot = jnp.stack([-tr[..., 1, :], tr[..., 0, :]], axis=-2).reshape(t.shape)
    return t * cos + rot * sin


def retention_scan(q, k, v, log_gamma, s0, strict):
    b, h, l, _ = q.shape
    dt = q.dtype
    n_chunks = l // RET_CHUNK
    lg = log_gamma.astype(jnp.float32)
    idx = jnp.arange(RET_CHUNK, dtype=jnp.float32)
    diff = idx[:, None] - idx[None, :]
    mask = (diff > 0) if strict else (diff >= 0)
    decay = jnp.where(mask, jnp.exp(jnp.where(mask, diff, 0.0) * lg[:, None, None]), 0.0).astype(dt)
    q_decay = jnp.exp((idx + 1.0) * lg[:, None]).astype(dt)[..., None]
    k_decay = jnp.exp((RET_CHUNK - 1.0 - idx) * lg[:, None]).astype(dt)[..., None]
    chunk_decay = jnp.exp(RET_CHUNK * lg).astype(dt)[:, None, None]

    def to_chunks(t):
        return jnp.moveaxis(t.reshape(b, h, n_chunks, RET_CHUNK, t.shape[-1]), 2, 0)

    def step(s, qkv):
        qc, kc, vc = qkv
        scores = jnp.einsum('bhqd,bhkd->bhqk', qc, kc) * decay
        out = (jnp.einsum('bhqk,bhkv->bhqv', scores, vc)
               + jnp.einsum('bhqd,bhdv->bhqv', qc, s) * q_decay)
        s = s * chunk_decay + jnp.einsum('bhkd,bhkv->bhdv', kc * k_decay, vc)
        return s, out

    _, out = lax.scan(step, s0.astype(dt), (to_chunks(q), to_chunks(k), to_chunks(v)))
    return jnp.moveaxis(out, 0, 2).reshape(b, h, l, v.shape[-1])


def retention_state(k, v, log_gamma, reverse):
    l = k.shape[2]
    pos = jnp.arange(l, dtype=jnp.float32)
    expo = pos if reverse else (l - 1.0 - pos)
    w = jnp.exp(expo[None, :] * log_gamma.astype(jnp.float32)[:, None]).astype(k.dtype)
    return jnp.einsum('hl,bhld,bhle->bhde', w, k, v)


def bidir_retention(q, k, v, lg_f, lg_b, s0_f, s0_b):
    out_f = retention_scan(q, k, v, lg_f, s0_f, False)
    flip = lambda t: jnp.flip(t, axis=2)
    out_b = retention_scan(flip(q), flip(k), flip(v), lg_b, s0_b, True)
    return out_f + flip(out_b)


def spatial_gating(u, v, ln_g, ln_b, w_s, b_s):
    b, l, _ = u.shape
    u = jax.nn.gelu(u)
    v32 = jax.nn.gelu(v).astype(jnp.float32).reshape(b, l, SGU_GROUPS, SGU_GROUP_DIM)
    mu = jnp.mean(v32, axis=-1, keepdims=True)
    var = jnp.mean(jnp.square(v32 - mu), axis=-1, keepdims=True)
    vn = ((v32 - mu) * lax.rsqrt(var + EPS)).astype(u.dtype)
    vn = vn * ln_g.reshape(SGU_GROUPS, SGU_GROUP_DIM) + ln_b.reshape(SGU_GROUPS, SGU_GROUP_DIM)
    vn = vn.reshape(b, l // SGU_CHUNK, SGU_CHUNK, SGU_GROUPS, SGU_GROUP_DIM)
    mixed = jnp.einsum('gpq,bnqgc->bnpgc', w_s, vn) + b_s.T[:, :, None]
    return u * mixed.reshape(b, l, SGU_W)


def token_mixer(p, rope, lg_f, lg_b, s0_f, s0_b, gn_g, ln_g, ln_b, w_s, b_s):
    b, l, _ = p.shape
    q = split_heads(p[..., Q_OFF:K_OFF], RET_HEADS)
    k = split_heads(p[..., K_OFF:V_OFF], RET_HEADS) * RET_DK ** -0.5
    v = split_heads(p[..., V_OFF:G_OFF], RET_HEADS)
    if rope is not None:
        q = rope_2d(q, *rope)
        k = rope_2d(k, *rope)
    y32 = bidir_retention(q, k, v, lg_f, lg_b, s0_f, s0_b).astype(jnp.float32)
    mu = jnp.mean(y32, axis=-1, keepdims=True)
    var = jnp.mean(jnp.square(y32 - mu), axis=-1, keepdims=True)
    y = ((y32 - mu) * lax.rsqrt(var + EPS)).astype(p.dtype)
    y = y.transpose(0, 2, 1, 3).reshape(b, l, RET_W) * gn_g
    ret_out = jax.nn.silu(p[..., G_OFF:U_OFF]) * y
    sgu_out = spatial_gating(p[..., U_OFF:SV_OFF], p[..., SV_OFF:IN_W], ln_g, ln_b, w_s, b_s)
    return jnp.concatenate([ret_out, sgu_out], axis=-1)


def grouped_experts(hf, e_flat, tok_flat, w_flat, w_gate, w_up, w_down):
    m = e_flat.shape[0]
    n_tok = hf.shape[0]
    n_blocks = -(-m // MOE_BLOCK) + N_EXPERTS
    n_rows = n_blocks * MOE_BLOCK
    order = jnp.argsort(e_flat)
    e_sorted = e_flat[order]
    counts = jax.ops.segment_sum(jnp.ones_like(e_flat), e_flat, num_segments=N_EXPERTS)
    starts = jnp.cumsum(counts) - counts
    padded = (counts + MOE_BLOCK - 1) // MOE_BLOCK * MOE_BLOCK
    pends = jnp.cumsum(padded)
    pstarts = pends - padded
    dest = pstarts[e_sorted] + jnp.arange(m, dtype=e_flat.dtype) - starts[e_sorted]
    row_tok = jnp.zeros((n_rows,), jnp.int32).at[dest].set(tok_flat[order])
    row_w = jnp.zeros((n_rows,), w_flat.dtype).at[dest].set(w_flat[order])
    block_e = jnp.minimum(jnp.searchsorted(pends, jnp.arange(n_blocks) * MOE_BLOCK, side='right'),
                          N_EXPERTS - 1)

    def expert_block(args):
        tok, e = args
        xb = hf[tok]
        hid = jax.nn.silu(xb @ w_gate[e]) * (xb @ w_up[e])
        return hid @ w_down[e]

    out = lax.map(expert_block, (row_tok.reshape(n_blocks, MOE_BLOCK), block_e))
    out = out.reshape(n_rows, -1) * row_w[:, None]
    return jax.ops.segment_sum(out, row_tok, num_segments=n_tok)


def hier_moe(h, w_rg, b_rg, w_re, b_re, w_gate, w_up, w_down):
    b, l, d = h.shape
    n_tok = b * l
    hf = h.reshape(n_tok, d)
    g_logits = (hf @ w_rg + b_rg).astype(jnp.float32)
    g_prob = jax.nn.softmax(g_logits, axis=-1)
    g_sel = jnp.argmax(g_logits, axis=-1).astype(jnp.int32)
    p_g = jnp.take_along_axis(g_prob, g_sel[:, None], axis=-1)
    e_logits = (hf @ w_re + b_re).astype(jnp.float32).reshape(n_tok, MOE_GROUPS, EXPERTS_PER_GROUP)
    e_logits = jnp.take_along_axis(e_logits, g_sel[:, None, None], axis=1)[:, 0]
    top_v, top_i = lax.top_k(e_logits, MOE_TOP_K)
    p_e = jax.nn.softmax(top_v, axis=-1)
    weights = (p_g * p_e).astype(h.dtype)
    expert = g_sel[:, None] * EXPERTS_PER_GROUP + top_i.astype(jnp.int32)
    tok = jnp.repeat(jnp.arange(n_tok, dtype=jnp.int32), MOE_TOP_K)
    y = grouped_experts(hf, expert.reshape(-1), tok, weights.reshape(-1), w_gate, w_up, w_down)
    return y.reshape(b, l, d)


def setup_inputs(seed: int = 0) -> dict:
    key = jax.random.key(seed)
    ks = jax.random.split(key, 25)
    f32 = jnp.float32
    nrm = lambda k, shape, s: jax.random.normal(k, shape, f32) * s
    heads = jnp.arange(RET_HEADS, dtype=f32)
    decay_raw = jnp.log(-jnp.log1p(-jnp.exp2(-5.0 - heads)))
    return {
        'x': nrm(ks[0], (BATCH, SEQ, D_MODEL), 1.0),
        'c': nrm(ks[1], (BATCH, D_MODEL), 1.0),
        'ctx': nrm(ks[2], (BATCH, CTX_LEN, D_MODEL), 1.0),
        'c_ctx': nrm(ks[3], (D_MODEL,), 1.0),
        'w_ada': nrm(ks[4], (DEPTH, D_MODEL, N_MOD * D_MODEL), 0.5 * D_MODEL ** -0.5),
        'b_ada': nrm(ks[5], (DEPTH, N_MOD * D_MODEL), 0.02),
        'norm1_g': 1.0 + nrm(ks[6], (DEPTH, D_MODEL), 0.02),
        'norm2_g': 1.0 + nrm(ks[7], (DEPTH, D_MODEL), 0.02),
        'w_in': nrm(ks[8], (DEPTH, D_MODEL, IN_W), D_MODEL ** -0.5),
        'ret_decay_f': decay_raw + nrm(ks[9], (DEPTH, RET_HEADS), 0.05),
        'ret_decay_b': decay_raw + nrm(ks[10], (DEPTH, RET_HEADS), 0.05),
        'ret_gn_g': 1.0 + nrm(ks[11], (DEPTH, RET_W), 0.02),
        'sgu_ln_g': 1.0 + nrm(ks[12], (DEPTH, SGU_W), 0.02),
        'sgu_ln_b': nrm(ks[13], (DEPTH, SGU_W), 0.02),
        'sgu_w_s': nrm(ks[14], (DEPTH, SGU_GROUPS, SGU_CHUNK, SGU_CHUNK), SGU_CHUNK ** -0.5),
        'sgu_b_s': 1.0 + nrm(ks[15], (DEPTH, SGU_GROUPS, SGU_CHUNK), 0.1),
        'w_out': nrm(ks[16], (DEPTH, MIX_W, D_MODEL), MIX_W ** -0.5),
        'w_router_group': nrm(ks[17], (DEPTH, D_MODEL, MOE_GROUPS), D_MODEL ** -0.5),
        'b_router_group': nrm(ks[18], (DEPTH, MOE_GROUPS), 0.01),
        'w_router_expert': nrm(ks[19], (DEPTH, D_MODEL, N_EXPERTS), D_MODEL ** -0.5),
        'b_router_expert': nrm(ks[20], (DEPTH, N_EXPERTS), 0.01),
        'w_gate': nrm(ks[21], (DEPTH, N_EXPERTS, D_MODEL, D_EXPERT), D_MODEL ** -0.5),
        'w_up': nrm(ks[22], (DEPTH, N_EXPERTS, D_MODEL, D_EXPERT), D_MODEL ** -0.5),
        'w_down': nrm(ks[23], (DEPTH, N_EXPERTS, D_EXPERT, D_MODEL), D_EXPERT ** -0.5),
        'final_g': 1.0 + nrm(ks[24], (D_MODEL,), 0.02),
    }


def reference(x, c, ctx, c_ctx, w_ada, b_ada, norm1_g, norm2_g, w_in, ret_decay_f, ret_decay_b,
              ret_gn_g, sgu_ln_g, sgu_ln_b, sgu_w_s, sgu_b_s, w_out, w_router_group, b_router_group,
              w_router_expert, b_router_expert, w_gate, w_up, w_down, final_g):
    n_lat = x.shape[1]
    rope = rope_tables(n_lat, x.dtype)
    silu_c = jax.nn.silu(c)
    silu_cc = jax.nn.silu(c_ctx)
    for layer in range(DEPTH):
        mod = (silu_c @ w_ada[layer] + b_ada[layer])[:, None, :]
        sh1, sc1, g1, sh2, sc2, g2 = jnp.split(mod, N_MOD, axis=-1)
        mod_c = silu_cc @ w_ada[layer] + b_ada[layer]
        csh1, csc1, cg1, csh2, csc2, cg2 = jnp.split(mod_c, N_MOD, axis=-1)
        lg_f = -jnp.exp(ret_decay_f[layer])
        lg_b = -jnp.exp(ret_decay_b[layer])
        mixer_params = (ret_gn_g[layer], sgu_ln_g[layer], sgu_ln_b[layer], sgu_w_s[layer], sgu_b_s[layer])
        moe_params = (w_router_group[layer], b_router_group[layer], w_router_expert[layer],
                      b_router_expert[layer], w_gate[layer], w_up[layer], w_down[layer])

        hc = modulate(rms_norm(ctx, norm1_g[layer]), csh1, csc1)
        kc = split_heads(hc @ w_in[layer, :, K_OFF:V_OFF], RET_HEADS) * RET_DK ** -0.5
        vc = split_heads(hc @ w_in[layer, :, V_OFF:G_OFF], RET_HEADS)
        s_f = retention_state(kc, vc, lg_f, False)
        s_b = retention_state(kc, vc, lg_b, True)

        h = modulate(rms_norm(x, norm1_g[layer]), sh1, sc1)
        mix = token_mixer(h @ w_in[layer], rope, lg_f, lg_b, s_f, s_b, *mixer_params)
        x = x + g1 * (mix @ w_out[layer])
        h = modulate(rms_norm(x, norm2_g[layer]), sh2, sc2)
        x = x + g2 * hier_moe(h, *moe_params)

        if layer < DEPTH - 1:
            zero_state = jnp.zeros_like(s_f)
            mix_c = token_mixer(hc @ w_in[layer], None, lg_f, lg_b, zero_state, zero_state, *mixer_params)
            ctx = ctx + cg1 * (mix_c @ w_out[layer])
            hc2 = modulate(rms_norm(ctx, norm2_g[layer]), csh2, csc2)
            ctx = ctx + cg2 * hier_moe(hc2, *moe_params)
    return rms_norm(x, final_g)
```

```python
import numpy as np
import contextlib
import concourse.bass as bass
import concourse.mybir as mybir
from concourse.bass_utils import run_bass_kernel_spmd

F32 = mybir.dt.float32
BF16 = mybir.dt.bfloat16
I32 = mybir.dt.int32
AF = mybir.ActivationFunctionType
ALU = mybir.AluOpType
AX = mybir.AxisListType

NCORES = 8
D = 4096
SEQ = 8192
NCH = SEQ // 128
CTX = 256
HW = 1536
EPS = 1e-6
CAP = 1024
TSH = SEQ // NCORES
NEXP = 64


class DSem:
    def __init__(self, h):
        self.h = h
        self.count = 0


class Sch:
    CE = ["tensor", "vector", "scalar", "gpsimd"]
    ALL = ["tensor", "vector", "scalar", "gpsimd", "sync"]

    def __init__(self, nc, st):
        self.nc = nc
        self.st = st
        self.sem = {e: st.enter_context(nc.semaphore("es_" + e)) for e in self.CE}
        self.cnt = {e: 0 for e in self.CE}
        self.waited = {}
        self.q = {e: [] for e in self.ALL}
        self.dsems = []
        self.nds = 0

    def dsem(self):
        self.nds += 1
        d = DSem(self.st.enter_context(self.nc.semaphore("ds%d" % self.nds)))
        self.dsems.append(d)
        return d

    def op(self, eng, fn, deps=()):
        self.cnt[eng] += 1
        self.q[eng].append(([d for d in deps if d is not None], fn, self.sem[eng], 1))
        return (self.sem[eng], self.cnt[eng])

    def dma(self, eng, fn, ds, deps=(), inc=16):
        ds.count += inc
        self.q[eng].append(([d for d in deps if d is not None], fn, ds.h, inc))
        return (ds.h, ds.count)

    def all_tokens(self):
        toks = [(self.sem[e], self.cnt[e]) for e in self.CE if self.cnt[e] > 0]
        toks += [(d.h, d.count) for d in self.dsems if d.count > 0]
        return toks

    def run_phase(self):
        fin = self.all_tokens()
        nc = self.nc
        q = self.q
        self.q = {e: [] for e in self.ALL}

        def replay(ename):
            def f(eng):
                for deps, fn, sem, inc in q[ename]:
                    for (s, v) in deps:
                        key = (ename, id(s))
                        if self.waited.get(key, 0) < v:
                            eng.wait_ge(s, v)
                            self.waited[key] = v
                    ins = fn(eng)
                    ins.then_inc(sem, inc)
                for (s, v) in fin:
                    key = (ename, id(s))
                    if self.waited.get(key, 0) < v:
                        eng.wait_ge(s, v)
                        self.waited[key] = v
            return f

        with nc.Block() as block:
            block.tensor(replay("tensor"))
            block.vector(replay("vector"))
            block.scalar(replay("scalar"))
            block.gpsimd(replay("gpsimd"))
            block.sync(replay("sync"))


class Haz:
    def __init__(self, S):
        self.S = S
        self.w = {}
        self.r = {}

    def deps(self, reads, writes, eng=None):
        d = []
        for n in reads:
            d += list(self.w.get(n, {}).values())
        for n in writes:
            d += list(self.w.get(n, {}).values())
            d += list(self.r.get(n, {}).values())
        if eng == "tensor":
            own = id(self.S.sem["tensor"])
            d = [t for t in d if id(t[0]) != own]
        return d

    def commit(self, tok, reads, writes):
        for n in reads:
            self.r.setdefault(n, {})[id(tok[0])] = tok
        for n in writes:
            self.w[n] = {id(tok[0]): tok}
            self.r[n] = {}

    def op(self, eng, fn, reads=(), writes=(), extra=()):
        tok = self.S.op(eng, fn, self.deps(reads, writes, eng) + list(extra))
        self.commit(tok, reads, writes)
        return tok

    def dma(self, eng, fn, ds, reads=(), writes=(), extra=(), inc=16):
        tok = self.S.dma(eng, fn, ds, self.deps(reads, writes) + list(extra), inc=inc)
        self.commit(tok, reads, writes)
        return tok


def mod_segs(B0, nblk):
    out = []
    b = B0
    while b < B0 + nblk:
        r, k = divmod(b, 24)
        n = min(24 - k, B0 + nblk - b)
        out.append((r, k, n, b - B0))
        b += n
    return out


def build(dbg=None, nchunks=NCH):
    nc = bass.Bass("TRN2", target_bir_lowering=False)
    dbg = dbg or {}

    def din(name, shape, dt=F32):
        if name in dbg.get("tiny", ()):
            shape = [1, 1]
        return nc.dram_tensor(name, list(shape), dt, kind="ExternalInput")

    def dscr(name, shape, dt=F32):
        if name in dbg.get("inputs", ()):
            return nc.dram_tensor(name, list(shape), dt, kind="ExternalInput")
        if name in dbg:
            return nc.dram_tensor(name, list(shape), dt, kind="ExternalOutput")
        return nc.dram_tensor(name, list(shape), dt)

    xsh_d = din("xsh", [TSH, D])
    ctx_d = din("ctx", [CTX, D])
    cvecT_d = din("cvecT", [128, 64])
    wada_d = din("wada", [D, 3072])
    bada_d = din("bada", [1, 3072])
    g1T_d = din("g1T", [128, 32])
    gains_d = din("gains", [2, D])
    win_d = din("win", [128, 32, HW])
    dec_d = din("dec", [1, 2])
    v256_d = din("v256", [3, 256])
    wsT_d = din("wsT", [128, 128])
    bs_d = din("bs", [128, 1])
    woutsh_d = din("woutsh", [128, 4, D])
    wr_d = din("wr", [128, 32, 72])
    br_d = din("br", [1, 72])
    NEX = dbg.get("nexp", 8)
    wg_d = din("wg", [NEX, 128, 32, 512])
    wu_d = din("wu", [NEX, 128, 32, 512])
    wd_d = din("wd", [NEX, 128, 4, D])
    cst_d = din("cst", [128, 1024])
    sel_d = din("sel", [128, 8])
    idxm_d = din("idxm", [128, 32], I32)
    idxt_d = din("idxt", [128, 8], I32)
    cst2_d = din("cst2", [128, 4096 + CAP])
    out_d = nc.dram_tensor("out", [TSH, D], F32, kind="ExternalOutput")

    nocc = dbg.get("nocc", False)
    if nocc:
        xall_d = din("xall", [nchunks * 128, D])
        rope_d = din("rope", [NCH * 128, 1024])
    else:
        xall_d = dscr("xall", [SEQ, D])
        ropesh_d = din("ropesh", [TSH, 1024])
        ropein_d = dscr("ropein", [TSH, 1024])
        rope_d = dscr("ropeall", [SEQ, 1024])
    xin_d = dscr("xin", [TSH, D])
    woutin_d = dscr("woutin", [128, 4 * D])
    woutall_d = dscr("woutall", [NCORES * 128, 4 * D])
    dbgin_d = din("dbgin", [128, 512]) if dbg.get("skip0") else None
    modin_d = dscr("modin", [2, 3072])
    modall_d = dscr("modall", [16, 3072])
    yp_d = dscr("yp", [NCH, 128, 256])
    dB_d = dscr("dB", [NCH, 128, 512], BF16)
    sg_d = dscr("sg", [NCH, 128, 256], BF16)
    qT_d = dscr("qT", [NCH, 128, 256], BF16)
    mixT_d = dscr("mixT", [512, SEQ], BF16)
    mixTall_d = dscr("mixTall", [NCORES * 512, SEQ], BF16)
    x1_d = dscr("x1", [TSH, D])
    h2_d = dscr("h2", [TSH, D], BF16)
    h2all_d = dscr("h2all", [SEQ, D], BF16)
    R_d = dscr("R", [TSH, 132])
    Rall_d = dscr("Rall", [SEQ, 132])
    T_d = dscr("T", [SEQ, 16])
    lst_d = dscr("lst", [8 * CAP, 1], I32)
    NQ = dbg.get("nq", max(1, NEX // 2))
    Y_d = [dscr("Y%d" % q, [2 * CAP, D], BF16) for q in range(NQ)]
    Yall_d = [dscr("Yall%d" % q, [NCORES * 2 * CAP, D], BF16) for q in range(NQ)]
    Rdbg_d = nc.dram_tensor("Rdbg", [TSH, 132], F32, kind="ExternalOutput") if "Rdbg" in dbg else None
    dbgo_d = nc.dram_tensor("dbgo", [128, 4096], F32, kind="ExternalOutput") if dbg else None

    rg = [list(range(NCORES))]
    stop = dbg.get("stop", 99)

    with contextlib.ExitStack() as gst:
        S = Sch(nc, gst)
        ccsem = gst.enter_context(nc.semaphore("ccsem"))
        cc = DSem(ccsem)
        S.dsems.append(cc)
        ccw = DSem(gst.enter_context(nc.semaphore("ccwsem")))
        tok_wout = [None]
        pool = [S.dsem() for _ in range(40)]
        H = Haz(S)

        def T(st, name, shape, dt=F32):
            return st.enter_context(nc.sbuf_tensor("s_" + name, list(shape), dt))

        def P(st, name, shape, dt=F32):
            return st.enter_context(nc.psum_tensor("p_" + name, list(shape), dt))

        def collective(kind, src, dst, deps, sem=None):
            return S.dma("gpsimd", lambda g: g.collective_compute(
                kind, ALU.bypass, replica_groups=rg, ins=[src.ap().opt()], outs=[dst.ap().opt()]), sem or cc, deps,
                inc=1)

        cst = T(gst, "cst", [128, 1024])
        ident_f = cst[:, 0:128]
        identb = T(gst, "identb", [128, 128], BF16)
        dect = T(gst, "dect", [128, 8])
        DT = T(gst, "DTm", [128, 128])
        a1T = T(gst, "a1T", [128, 32])
        sh1T = T(gst, "sh1T", [128, 32])
        ca1T = T(gst, "ca1T", [128, 32])
        csh1T = T(gst, "csh1T", [128, 32])
        S0 = T(gst, "S0", [128, 1024])
        ctxw = T(gst, "ctxw", [128, 4])

        with contextlib.ExitStack() as st:
          if not dbg.get('skip0'):
            dl = pool[0:6]
            cvT = T(st, "cvT", [128, 64])
            sT = T(st, "sT", [128, 64])
            wa = [T(st, "wa%d" % i, [128, 3072]) for i in range(3)]
            bad = T(st, "bad", [2, 3072])
            modl = T(st, "modl", [2, 3072])
            mp = P(st, "mp", [2, 3072])
            lgt = T(st, "lgt", [128, 2])
            tmpa = T(st, "tmpa", [128, 512])
            g1T = T(st, "g1T", [128, 32])
            rows = T(st, "rows", [32, 512])
            tp = P(st, "tp0", [128, 128])

            t_c = S.dma("sync", lambda e: e.dma_start(out=cst[:], in_=cst_d.ap()), dl[0])
            t_cv = S.dma("sync", lambda e: e.dma_start(out=cvT[:], in_=cvecT_d.ap()), dl[0])
            t_b0 = S.dma("sync", lambda e: e.dma_start(out=bad[0:1, :], in_=bada_d.ap()), dl[0])
            t_b1 = S.dma("sync", lambda e: e.dma_start(out=bad[1:2, :], in_=bada_d.ap()), dl[0])
            t_g1 = S.dma("sync", lambda e: e.dma_start(out=g1T[:], in_=g1T_d.ap()), dl[0])
            t_dec = S.dma("sync", lambda e: e.dma_start(
                out=lgt[:], in_=bass.AP(dec_d, 0, [[0, 128], [1, 2]])), dl[0])
            t_s = S.op("scalar", lambda e: e.activation(out=sT[:], in_=cvT[:], func=AF.Silu), [t_b1])
            t_idb = S.op("vector", lambda e: e.tensor_copy(out=identb[:], in_=ident_f), [t_dec])
            t_lg = S.op("scalar", lambda e: e.activation(out=lgt[:], in_=lgt[:], func=AF.Exp), [t_dec, t_s])
            t_lg = S.op("vector", lambda e: e.tensor_scalar(out=lgt[:], in0=lgt[:], scalar1=-1.0, scalar2=None,
                                                           op0=ALU.mult), [t_lg, t_idb])
            t_d1 = S.op("scalar", lambda e: e.activation(out=dect[:, 0:3], in_=cst[:, 128:131], func=AF.Exp,
                                                         scale=lgt[:, 0:1]), [t_lg])
            t_d2 = S.op("scalar", lambda e: e.activation(out=dect[:, 3:6], in_=cst[:, 131:134], func=AF.Exp,
                                                         scale=lgt[:, 1:2]), [t_d1])
            t_d3 = S.op("scalar", lambda e: e.activation(out=ctxw[:, 0:2], in_=cst[:, 134:136], func=AF.Exp,
                                                         scale=lgt[:, 0:1]), [t_d2])
            t_d4 = S.op("scalar", lambda e: e.activation(out=ctxw[:, 2:4], in_=cst[:, 136:138], func=AF.Exp,
                                                         scale=lgt[:, 1:2]), [t_d3])
            t_d5 = S.op("vector", lambda e: e.tensor_scalar(out=ctxw[:], in0=ctxw[:], scalar1=1.0 / 16, scalar2=None,
                                                           op0=ALU.mult), [t_d4])
            t_e1 = S.op("scalar", lambda e: e.activation(out=tmpa[:, 0:128], in_=cst[:, 256:384], func=AF.Exp,
                                                         scale=lgt[:, 0:1]), [t_d4])
            t_e2 = S.op("scalar", lambda e: e.activation(out=tmpa[:, 128:256], in_=cst[:, 512:640], func=AF.Exp,
                                                         scale=lgt[:, 1:2]), [t_e1])
            t_e3 = S.op("vector", lambda e: e.tensor_tensor(out=tmpa[:, 0:128], in0=tmpa[:, 0:128],
                                                           in1=cst[:, 384:512], op=ALU.mult), [t_e2, t_d5])
            t_e4 = S.op("vector", lambda e: e.tensor_tensor(out=tmpa[:, 128:256], in0=tmpa[:, 128:256],
                                                           in1=cst[:, 640:768], op=ALU.mult), [t_e3])
            t_e5 = S.op("vector", lambda e: e.tensor_tensor(out=DT[:], in0=tmpa[:, 0:128], in1=tmpa[:, 128:256],
                                                           op=ALU.add), [t_e4])
            tl = [None] * 32
            tmm = [None] * 32
            for kc in range(32):
                b = kc % 3
                tl[kc] = S.dma("sync", (lambda e, kc=kc, b=b: e.dma_start(
                    out=wa[b][:], in_=wada_d[kc * 128:(kc + 1) * 128, :])), dl[1 + b],
                    [tmm[kc - 3]] if kc >= 3 else [])
                last = None
                for n in range(6):
                    last = S.op("tensor", (lambda e, kc=kc, b=b, n=n: e.matmul(
                        mp[:, n * 512:(n + 1) * 512], sT[:, kc * 2:kc * 2 + 2], wa[b][:, n * 512:(n + 1) * 512],
                        start=(kc == 0), stop=(kc == 31))), [tl[kc], t_s])
                tmm[kc] = last
            t_m = S.op("vector", lambda e: e.tensor_tensor(out=modl[:], in0=mp[:], in1=bad[:], op=ALU.add),
                       [tmm[31], t_b1, t_e5])
            t_mo = S.dma("sync", lambda e: e.dma_start(out=modin_d.ap(), in_=modl[:]), dl[4], [t_m])
            t_ag = collective("AllGather", modin_d, modall_d, [t_mo])

            ma = modall_d.ap().rearrange("(r m) (k f) -> r m k f", m=2, f=128)
            tr = []
            for v, (m, B0) in enumerate([(0, 0), (0, 32), (1, 0), (1, 32)]):
                for (r, k0, n, d0) in mod_segs(B0, 32):
                    tr.append(S.dma("sync", (lambda e, v=v, m=m, r=r, k0=k0, n=n, d0=d0: e.dma_start(
                        out=rows[d0:d0 + n, v * 128:(v + 1) * 128], in_=ma[r, m, k0:k0 + n, :])), dl[5], [t_ag]))
            outs = [sh1T, a1T, csh1T, ca1T]
            tprev = t_m
            for v in range(4):
                t1 = S.op("tensor", (lambda e, v=v: e.matmul(tp[:, 0:32], rows[:, v * 128:(v + 1) * 128],
                                                             ident_f[0:32, 0:32], start=True, stop=True)),
                          tr + [tprev])
                if v % 2 == 0:
                    tprev = S.op("vector", (lambda e, v=v: e.tensor_copy(out=outs[v][:], in_=tp[:, 0:32])), [t1, t_g1])
                else:
                    tprev = S.op("vector", (lambda e, v=v: e.scalar_tensor_tensor(
                        out=outs[v][:], in0=tp[:, 0:32], scalar=1.0, in1=g1T[:], op0=ALU.add, op1=ALU.mult)),
                        [t1, t_g1])
            if dbg.get("dump") == "p0":
                tq = S.op("vector", lambda e: e.tensor_copy(out=tmpa[:, 0:32], in_=sh1T[:]), [tprev])
                tq = S.op("vector", lambda e: e.tensor_copy(out=tmpa[:, 32:64], in_=a1T[:]), [tq])
                tq = S.op("vector", lambda e: e.tensor_copy(out=tmpa[:, 64:96], in_=csh1T[:]), [tq])
                tq = S.op("vector", lambda e: e.tensor_copy(out=tmpa[:, 96:128], in_=ca1T[:]), [tq])
                tq = S.op("vector", lambda e: e.tensor_copy(out=tmpa[:, 128:136], in_=dect[:]), [tq])
                tq = S.op("vector", lambda e: e.tensor_copy(out=tmpa[:, 136:140], in_=ctxw[:]), [tq])
                tq = S.op("vector", lambda e: e.tensor_copy(out=tmpa[:, 256:384], in_=DT[:]), [tq])
                S.dma("sync", lambda e: e.dma_start(out=dbgo_d[:, 0:512], in_=tmpa[:]), dl[5], [tq])
            S.run_phase()
        if dbg.get("skip0"):
            with contextlib.ExitStack() as st:
                tmpb = T(st, "tmpb", [128, 512])
                ta = S.dma("sync", lambda e: e.dma_start(out=cst[:], in_=cst_d.ap()), pool[0])
                tb = S.dma("sync", lambda e: e.dma_start(out=tmpb[:], in_=dbgin_d.ap()), pool[0])
                for dst, lo, hi in [(sh1T, 0, 32), (a1T, 32, 64), (csh1T, 64, 96), (ca1T, 96, 128),
                                    (dect, 128, 136), (ctxw, 136, 140), (DT, 256, 384)]:
                    S.op("vector", (lambda e, dst=dst, lo=lo, hi=hi: e.tensor_copy(out=dst[:], in_=tmpb[:, lo:hi])),
                         [ta, tb])
                S.op("vector", lambda e: e.tensor_copy(out=identb[:], in_=ident_f), [ta, tb])
                S.run_phase()
        if not nocc:
            t1 = S.dma("gpsimd", lambda e: e.dma_start(out=xin_d.ap(), in_=xsh_d.ap()), pool[0])
            t4 = collective("AllGather", xin_d, xall_d, [t1])
            if "ropesh" not in dbg.get("tiny", ()):
                t2b = S.dma("gpsimd", lambda e: e.dma_start(out=ropein_d.ap(), in_=ropesh_d.ap()), pool[0])
                t4 = collective("AllGather", ropein_d, rope_d, [t4, t2b])
            if "woutsh" not in dbg.get("tiny", ()):
                t2 = S.dma("gpsimd", lambda e: e.dma_start(
                    out=woutin_d.ap(), in_=woutsh_d.ap().rearrange("p a n -> p (a n)")), pool[0])
                tok_wout[0] = collective("AllGather", woutin_d, woutall_d, [t4, t2], sem=ccw)
            S.run_phase()
        if stop <= 0:
            return nc

        if not dbg.get('skipAB'):
            with contextlib.ExitStack() as st:
                W = T(st, "W", [128, 32, HW], BF16)
                xt = [T(st, "xt%d" % i, [128, D]) for i in range(2)]
                xs = T(st, "xs", [128, D], BF16)
                hT = [T(st, "hT%d" % i, [128, 32, 128], BF16) for i in range(2)]
                rt = [T(st, "rt%d" % i, [128, 1024]) for i in range(2)]
                ss = T(st, "ss", [128, 8])
                rA = T(st, "rA", [128, 512])
                rB = T(st, "rB", [128, 512])
                qk = T(st, "qk", [128, 512], BF16)
                qkT = T(st, "qkT", [128, 512], BF16)
                v16 = T(st, "v16", [128, 256], BF16)
                vf = T(st, "vf", [128, 256], BF16)
                vbk = T(st, "vbk", [128, 256], BF16)
                STm = T(st, "STm", [128, 128], BF16)
                pin = T(st, "pin", [128, 256])
                ypt = T(st, "ypt", [128, 256])
                Sf = T(st, "Sf", [128, 512])
                Sf16 = [T(st, "Sf16_%d" % i, [128, 512], BF16) for i in range(2)]
                dBt = T(st, "dBt", [128, 512], BF16)
                sgt = T(st, "sgt", [128, 256], BF16)
                gu = T(st, "gu", [128, 512])
                cen = T(st, "cen", [128, 256])
                junk = T(st, "junk", [128, 256])
                vn16 = T(st, "vn16", [128, 256], BF16)
                so16 = T(st, "so16", [128, 256], BF16)
                soT = T(st, "soT", [128, 256], BF16)
                lnrow = T(st, "lnrow", [128, 768])
                wsf = T(st, "wsf", [128, 128])
                ws16 = T(st, "ws16", [128, 128], BF16)
                bst = T(st, "bst", [128, 1])
                pp = [P(st, "pp%d" % i, [128, 512]) for i in range(3)]
                tpA = P(st, "tpA", [128, 1024], BF16)
                tpB = P(st, "tpB", [128, 1024], BF16)
                m5 = P(st, "m5", [128, 512])
                m6 = P(st, "m6", [128, 512])
                pst = P(st, "pst", [128, 512])
                scT = m5[:, 0:128]
                pint = m6[:, 0:256]
                pf = pst[:, 0:256]
                pmx = pst[:, 0:256]
                dl = pool[0:12]

                for kc in range(32):
                    H.dma("gpsimd", (lambda e, kc=kc: e.dma_start(out=W[:, kc, :], in_=win_d[:, kc, :])), dl[0],
                          writes=["W"] if kc == 0 else [], extra=[])
                tW = (dl[0].h, dl[0].count)
                H.commit(tW, [], ["W"])
                H.dma("sync", lambda e: e.dma_start(out=lnrow[:], in_=bass.AP(v256_d, 0, [[0, 128], [1, 768]])), dl[1],
                      writes=["lnrow"])
                H.dma("sync", lambda e: e.dma_start(out=wsf[:], in_=wsT_d.ap()), dl[1], writes=["wsf"])
                H.dma("sync", lambda e: e.dma_start(out=bst[:], in_=bs_d.ap()), dl[1], writes=["bst"])
                H.commit((dl[1].h, dl[1].count), [], ["lnrow", "wsf", "bst"])
                H.op("vector", lambda e: e.tensor_copy(out=ws16[:], in_=wsf[:]), reads=["wsf"], writes=["ws16"])

                def front(src_ap, slot, aT, shT, ldsem):
                    front_a(src_ap, slot, ldsem)
                    front_b(slot, aT, shT)

                def front_a(src_ap, slot, ldsem):
                    b = slot
                    H.dma("sync", lambda e: e.dma_start(out=xt[b][:], in_=src_ap), ldsem, writes=["xt%d" % b])
                    flvl = dbg.get("flvl", 9)
                    if flvl < 2:
                        return
                    H.op("scalar", lambda e: e.memzero(ss[:, 0:1]), writes=["ss0"])
                    H.op("scalar", lambda e: e.activation(out=xs[:], in_=xt[b][:], func=AF.Square, accum_out=ss[:, 0:1]),
                         reads=["xt%d" % b], writes=["xs", "ss0"])
                    if flvl < 3:
                        return
                    H.op("scalar", lambda e: e.activation(out=ss[:, 1:2], in_=ss[:, 0:1], func=AF.Sqrt, scale=1.0 / D, bias=EPS),
                         reads=["ss0"], writes=["ss1"])
                    H.op("vector", lambda e: e.reciprocal(out=ss[:, 1:2], in_=ss[:, 1:2]), reads=["ss1"], writes=["ss1"])
                    if flvl < 4:
                        return
                    H.op("vector", lambda e: e.tensor_scalar(out=xs[:], in0=xt[b][:], scalar1=ss[:, 1:2], scalar2=None,
                                                             op0=ALU.mult), reads=["xt%d" % b, "ss1"], writes=["xs"])

                def front_b(slot, aT, shT):
                    b = slot
                    flvl = dbg.get("flvl", 9)
                    if flvl < 5:
                        return
                    for g4 in range(4):
                        tp = tpA if g4 % 2 == 0 else tpB
                        tpn = "tpA" if g4 % 2 == 0 else "tpB"
                        for j in range(8):
                            kc = g4 * 8 + j
                            H.op("tensor", (lambda e, kc=kc, j=j, tp=tp: e.transpose(
                                tp[:, j * 128:(j + 1) * 128], xs[:, kc * 128:(kc + 1) * 128], identb[:])),
                                reads=["xs"], writes=[tpn])
                        for j in range(8 if flvl >= 6 else 0):
                            kc = g4 * 8 + j
                            nm = "hT%d_%d" % (b, kc)
                            if g4 % 2 == 0:
                                H.op("vector", (lambda e, kc=kc, j=j, tp=tp: e.tensor_scalar(
                                    out=hT[b][:, kc, :], in0=tp[:, j * 128:(j + 1) * 128], scalar1=aT[:, kc:kc + 1],
                                    scalar2=shT[:, kc:kc + 1], op0=ALU.mult, op1=ALU.add)),
                                    reads=[tpn], writes=[nm])
                            else:
                                H.op("scalar", (lambda e, kc=kc, j=j, tp=tp: e.activation(
                                    out=hT[b][:, kc, :], in_=tp[:, j * 128:(j + 1) * 128], func=AF.Identity,
                                    bias=shT[:, kc:kc + 1], scale=aT[:, kc:kc + 1])),
                                    reads=[tpn], writes=[nm])

                def proj(slot, nts, kc0=0, kc1=32):
                    b = slot
                    for kc in range(kc0, kc1):
                        for nt in nts:
                            H.op("tensor", (lambda e, kc=kc, nt=nt: e.matmul(
                                pp[nt][:], hT[b][:, kc, :], W[:, kc, nt * 512:(nt + 1) * 512],
                                start=(kc == 0), stop=(kc == 31))),
                                reads=["hT%d_%d" % (b, kc), "W"], writes=["pp%d" % nt])

                lvl = dbg.get("lvl", 9)
                for c in range(2 if lvl >= 2 else 0):
                    front(ctx_d[c * 128:(c + 1) * 128, :], c, ca1T, csh1T, dl[2 + c])
                    if lvl < 3:
                        continue
                    proj(c, [0, 1])
                    if lvl < 4:
                        continue
                    H.op("scalar", lambda e: e.copy(out=qk[:, 256:512], in_=pp[0][:, 256:512]), reads=["pp0"], writes=["qk"])
                    H.op("vector", (lambda e, c=c: e.tensor_scalar(out=vf[:], in0=pp[1][:, 0:256], scalar1=ctxw[:, c:c + 1],
                                                                  scalar2=None, op0=ALU.mult)), reads=["pp1"], writes=["vf"])
                    H.op("vector", (lambda e, c=c: e.tensor_scalar(out=vbk[:], in0=pp[1][:, 0:256],
                                                                  scalar1=ctxw[:, 2 + c:3 + c], scalar2=None, op0=ALU.mult)),
                         reads=["pp1"], writes=["vbk"])
                    for dc in range(2):
                        H.op("tensor", (lambda e, dc=dc: e.matmul(
                            pst[:, dc * 256:(dc + 1) * 256], qk[:, 256 + dc * 128:384 + dc * 128], vf[:],
                            start=True, stop=True)), reads=["qk", "vf"], writes=["pst"])
                        H.op("tensor", (lambda e, dc=dc: e.matmul(
                            m6[:, dc * 256:(dc + 1) * 256], qk[:, 256 + dc * 128:384 + dc * 128], vbk[:],
                            start=True, stop=True)), reads=["qk", "vbk"], writes=["m6"])
                    if c == 0:
                        H.op("vector", lambda e: e.tensor_copy(out=S0[:, 0:512], in_=pst[:]), reads=["pst"], writes=["S0"])
                        H.op("vector", lambda e: e.tensor_copy(out=S0[:, 512:1024], in_=m6[:]), reads=["m6"], writes=["S0"])
                    else:
                        H.op("vector", lambda e: e.tensor_tensor(out=S0[:, 0:512], in0=S0[:, 0:512], in1=pst[:], op=ALU.add),
                             reads=["pst", "S0"], writes=["S0"])
                        H.op("vector", lambda e: e.tensor_tensor(out=S0[:, 512:1024], in0=S0[:, 512:1024], in1=m6[:],
                                                                op=ALU.add), reads=["m6", "S0"], writes=["S0"])
                H.op("vector", lambda e: e.tensor_copy(out=Sf[:], in_=S0[:, 0:512]), reads=["S0"], writes=["Sf"])
                H.op("scalar", lambda e: e.copy(out=Sf16[0][:], in_=S0[:, 0:512]), reads=["S0"], writes=["Sf16_0"])
                if dbg.get("dump") == "ctx":
                    H.dma("sync", lambda e: e.dma_start(out=dbgo_d[:, 0:1024], in_=S0[:]), dl[4], reads=["S0"])

                def back(n):
                    back1(n)
                    back2(n)

                def back1(n):
                    b = n % 2
                    p4 = pp[0][:].rearrange("p (g b f) -> p g b f", g=4, b=2)
                    rB4 = rB[:].rearrange("p (g b f) -> p g b f", g=4, b=2)
                    sn4 = rt[b][:, 512:1024].rearrange("p (g b f) -> p g b f", g=4, b=2)
                    H.op("vector", lambda e: e.tensor_tensor(out=rA[:], in0=pp[0][:], in1=rt[b][:, 0:512], op=ALU.mult),
                         reads=["pp0", "rt%d" % b], writes=["rA"])
                    H.op("vector", lambda e: e.tensor_tensor(out=rB4[:, :, 0, :], in0=p4[:, :, 1, :], in1=sn4[:, :, 0, :],
                                                            op=ALU.mult), reads=["pp0", "rt%d" % b], writes=["rB0"])
                    H.op("vector", lambda e: e.tensor_tensor(out=rB4[:, :, 1, :], in0=p4[:, :, 0, :], in1=sn4[:, :, 1, :],
                                                            op=ALU.mult), reads=["pp0", "rt%d" % b], writes=["rB1"])
                    H.op("vector", lambda e: e.tensor_tensor(out=qk[:], in0=rA[:], in1=rB[:], op=ALU.add),
                         reads=["rA", "rB0", "rB1"], writes=["qk"])
                    H.op("scalar", lambda e: e.copy(out=v16[:], in_=pp[1][:, 0:256]), reads=["pp1"], writes=["v16"])
                    H.op("scalar", lambda e: e.activation(out=vf[:], in_=pp[1][:, 0:256], func=AF.Identity, scale=dect[:, 1:2]),
                         reads=["pp1"], writes=["vf"])
                    H.op("scalar", lambda e: e.activation(out=vbk[:], in_=pp[1][:, 0:256], func=AF.Identity, scale=dect[:, 4:5]),
                         reads=["pp1"], writes=["vbk"])
                    H.op("scalar", lambda e: e.activation(out=sgt[:], in_=pp[1][:, 256:512], func=AF.Silu),
                         reads=["pp1"], writes=["sgt"])
                    H.op("scalar", lambda e: e.activation(out=gu[:], in_=pp[2][:], func=AF.Gelu_apprx_tanh),
                         reads=["pp2"], writes=["gu"])

                def back2(n, hooks=(None, None, None)):
                    b = n % 2
                    sfr = "Sf16_%d" % (n % 2)
                    sfw = "Sf16_%d" % ((n + 1) % 2)
                    for j in range(4):
                        H.op("tensor", (lambda e, j=j: e.transpose(tpA[:, j * 128:(j + 1) * 128],
                                                                   qk[:, j * 128:(j + 1) * 128], identb[:])),
                             reads=["qk"], writes=["tpA"])
                    H.op("vector", lambda e: e.tensor_copy(out=qkT[:], in_=tpA[:, 0:512]),
                         reads=["tpA"], writes=["qkT"])
                    if hooks[0]:
                        hooks[0]()
                    for dc in range(2):
                        H.op("tensor", (lambda e, dc=dc: e.matmul(scT, qkT[:, (2 + dc) * 128:(3 + dc) * 128],
                                                                  qkT[:, dc * 128:(dc + 1) * 128],
                                                                  start=(dc == 0), stop=(dc == 1))),
                             reads=["qkT"], writes=["m5"])
                    H.op("vector", lambda e: e.tensor_tensor(out=STm[:], in0=scT, in1=DT[:], op=ALU.mult),
                         reads=["m5"], writes=["STm"])
                    if hooks[1]:
                        hooks[1]()
                    H.op("tensor", lambda e: e.matmul(pint, STm[:], v16[:], start=True, stop=True),
                         reads=["STm", "v16"], writes=["m6"])
                    for dc in range(2):
                        H.op("tensor", (lambda e, dc=dc: e.matmul(pf, qkT[:, dc * 128:(dc + 1) * 128],
                                                                  Sf16[n % 2][:, dc * 256:(dc + 1) * 256],
                                                                  start=(dc == 0), stop=(dc == 1))),
                             reads=["qkT", sfr], writes=["pst"])
                    H.op("scalar", lambda e: e.copy(out=pin[:], in_=pint), reads=["m6"], writes=["pin"])
                    H.op("vector", lambda e: e.scalar_tensor_tensor(out=ypt[:], in0=pf, scalar=dect[:, 0:1], in1=pin[:],
                                                                   op0=ALU.mult, op1=ALU.add),
                         reads=["pst", "pin"], writes=["ypt"])
                    H.dma("sync", lambda e: e.dma_start(out=yp_d[n], in_=ypt[:]), dl[5], reads=["ypt"])
                    H.dma("sync", lambda e: e.dma_start(out=sg_d[n], in_=sgt[:]), dl[6], reads=["sgt"])
                    H.dma("sync", lambda e: e.dma_start(out=qT_d[n], in_=qkT[:, 0:256]), dl[7], reads=["qkT"])
                    for dc in range(2):
                        H.op("tensor", (lambda e, dc=dc: e.matmul(m5[:, dc * 256:(dc + 1) * 256],
                                                                  qk[:, 256 + dc * 128:384 + dc * 128], vf[:],
                                                                  start=True, stop=True)),
                             reads=["qk", "vf"], writes=["m5"])
                    H.op("vector", lambda e: e.scalar_tensor_tensor(out=Sf[:], in0=Sf[:], scalar=dect[:, 2:3], in1=m5[:],
                                                                   op0=ALU.mult, op1=ALU.add),
                         reads=["m5", "Sf"], writes=["Sf"])
                    H.op("scalar", lambda e: e.copy(out=Sf16[(n + 1) % 2][:], in_=Sf[:]), reads=["Sf"], writes=[sfw])
                    for dc in range(2):
                        H.op("tensor", (lambda e, dc=dc: e.matmul(m6[:, dc * 256:(dc + 1) * 256],
                                                                  qk[:, 256 + dc * 128:384 + dc * 128], vbk[:],
                                                                  start=True, stop=True)),
                             reads=["qk", "vbk"], writes=["m6"])
                    H.op("vector", lambda e: e.tensor_copy(out=dBt[:], in_=m6[:]), reads=["m6"], writes=["dBt"])
                    H.dma("sync", lambda e: e.dma_start(out=dB_d[n], in_=dBt[:]), dl[8], reads=["dBt"])
                    if hooks[2]:
                        hooks[2]()
                    H.op("vector", lambda e: e.reduce_sum(out=ss[:, 2:3], in_=gu[:, 256:512], axis=AX.X),
                         reads=["gu"], writes=["ss2"])
                    H.op("vector", lambda e: e.tensor_scalar(out=ss[:, 2:3], in0=ss[:, 2:3], scalar1=1.0 / 256, scalar2=None,
                                                             op0=ALU.mult), reads=["ss2"], writes=["ss2"])
                    H.op("vector", lambda e: e.tensor_scalar(out=cen[:], in0=gu[:, 256:512], scalar1=ss[:, 2:3], scalar2=None,
                                                             op0=ALU.subtract), reads=["gu", "ss2"], writes=["cen"])
                    H.op("scalar", lambda e: e.memzero(ss[:, 3:4]), writes=["ss3"])
                    H.op("scalar", lambda e: e.activation(out=junk[:], in_=cen[:], func=AF.Square, accum_out=ss[:, 3:4]),
                         reads=["cen"], writes=["junk", "ss3"])
                    H.op("scalar", lambda e: e.activation(out=ss[:, 4:5], in_=ss[:, 3:4], func=AF.Sqrt, scale=1.0 / 256, bias=EPS),
                         reads=["ss3"], writes=["ss4"])
                    H.op("vector", lambda e: e.reciprocal(out=ss[:, 4:5], in_=ss[:, 4:5]), reads=["ss4"], writes=["ss4"])
                    H.op("vector", lambda e: e.scalar_tensor_tensor(out=cen[:], in0=cen[:], scalar=ss[:, 4:5],
                                                                   in1=lnrow[:, 256:512], op0=ALU.mult, op1=ALU.mult),
                         reads=["cen", "ss4", "lnrow"], writes=["cen"])
                    H.op("vector", lambda e: e.tensor_tensor(out=vn16[:], in0=cen[:], in1=lnrow[:, 512:768], op=ALU.add),
                         reads=["cen", "lnrow"], writes=["vn16"])
                    H.op("tensor", lambda e: e.matmul(pmx, ws16[:], vn16[:], start=True, stop=True),
                         reads=["ws16", "vn16"], writes=["pst"])
                    H.op("vector", lambda e: e.scalar_tensor_tensor(out=so16[:], in0=pmx, scalar=bst[:, 0:1],
                                                                   in1=gu[:, 0:256], op0=ALU.add, op1=ALU.mult),
                         reads=["pst", "gu", "bst"], writes=["so16"])
                    for j in range(2):
                        H.op("tensor", (lambda e, j=j: e.transpose(tpB[:, j * 128:(j + 1) * 128],
                                                                   so16[:, j * 128:(j + 1) * 128], identb[:])),
                             reads=["so16"], writes=["tpB"])
                    H.op("scalar", lambda e: e.copy(out=soT[:], in_=tpB[:, 0:256]), reads=["tpB"], writes=["soT"])
                    for j in range(2):
                        H.dma("sync", (lambda e, j=j: e.dma_start(
                            out=mixT_d[256 + j * 128:384 + j * 128, n * 128:(n + 1) * 128],
                            in_=soT[:, j * 128:(j + 1) * 128])), dl[9], reads=["soT"])

                def frontx_a(n):
                    b = n % 2
                    H.dma("sync", lambda e: e.dma_start(out=rt[b][:], in_=rope_d[n * 128:(n + 1) * 128, :]), dl[10 + b], writes=["rt%d" % b])
                    front_a(xall_d[n * 128:(n + 1) * 128, :], b, dl[2 + b])

                nloop = 0 if dbg.get("nomain") else nchunks
                for n in range(min(2, nloop)):
                    frontx_a(n)
                    front_b(n % 2, a1T, sh1T)
                if nloop:
                    proj(0, [0, 1, 2])
                for n in range(nloop):
                    back1(n)
                    if n + 2 < nloop:
                        frontx_a(n + 2)
                    if n + 1 < nloop:
                        nb_ = (n + 1) % 2
                        hooks = ((lambda nb_=nb_: proj(nb_, [0, 1, 2], 0, 11)),
                                 (lambda nb_=nb_: proj(nb_, [0, 1, 2], 11, 22)),
                                 (lambda nb_=nb_: proj(nb_, [0, 1, 2], 22, 32)))
                    else:
                        hooks = (None, None, None)
                    back2(n, hooks)
                    if n + 2 < nloop:
                        front_b(n % 2, a1T, sh1T)
                if dbg.get("dump") == "A":
                    H.dma("sync", lambda e: e.dma_start(out=dbgo_d[:, 0:512], in_=Sf[:]), dl[4], reads=["Sf"])
                S.run_phase()
            if stop <= 1:
                return nc
            with contextlib.ExitStack() as st:
                Bs = T(st, "Bs", [128, 512])
                B16 = [T(st, "B16_%d" % i, [128, 512], BF16) for i in range(2)]
                qTt = [T(st, "qTt%d" % i, [128, 256], BF16) for i in range(2)]
                ypb = [T(st, "ypb%d" % i, [128, 256]) for i in range(2)]
                sgb = [T(st, "sgb%d" % i, [128, 256], BF16) for i in range(2)]
                dBb = [T(st, "dBb%d" % i, [128, 512], BF16) for i in range(2)]
                yv = T(st, "yv", [128, 256])
                junk2 = T(st, "junk2", [128, 256])
                ro16 = T(st, "ro16", [128, 256], BF16)
                roT = T(st, "roT", [128, 256], BF16)
                gnrow = T(st, "gnrow", [128, 256])
                st8 = T(st, "st8", [128, 8])
                pb = P(st, "pb", [128, 512])
                tpC = P(st, "tpC", [128, 1024], BF16)
                dl = pool[12:24]
                H.dma("sync", lambda e: e.dma_start(out=gnrow[:], in_=bass.AP(v256_d, 0, [[0, 128], [1, 256]])), dl[0],
                      writes=["gnrow"])
                H.op("vector", lambda e: e.tensor_copy(out=Bs[:], in_=S0[:, 512:1024]), reads=["S0"], writes=["Bs"])
                H.op("scalar", lambda e: e.copy(out=B16[0][:], in_=S0[:, 512:1024]), reads=["S0"], writes=["B16_0"])

                def loadB(n, b):
                    H.dma("sync", lambda e: e.dma_start(out=qTt[b][:], in_=qT_d[n]), dl[1 + b], writes=["qTt%d" % b])
                    H.dma("sync", lambda e: e.dma_start(out=ypb[b][:], in_=yp_d[n]), dl[3 + b], writes=["ypb%d" % b])
                    H.dma("sync", lambda e: e.dma_start(out=sgb[b][:], in_=sg_d[n]), dl[5 + b], writes=["sgb%d" % b])
                    H.dma("sync", lambda e: e.dma_start(out=dBb[b][:], in_=dB_d[n]), dl[7 + b], writes=["dBb%d" % b])

                order = list(range(nchunks - 1, -1, -1))
                loadB(order[0], 0)
                for it, n in enumerate(order):
                    b = it % 2
                    if it + 1 < len(order):
                        loadB(order[it + 1], (it + 1) % 2)
                    bn = "B16_%d" % (it % 2)
                    bw = "B16_%d" % ((it + 1) % 2)
                    for dc in range(2):
                        H.op("tensor", (lambda e, dc=dc, b=b, it=it: e.matmul(
                            pb[:, 0:256], qTt[b][:, dc * 128:(dc + 1) * 128], B16[it % 2][:, dc * 256:(dc + 1) * 256],
                            start=(dc == 0), stop=(dc == 1))), reads=["qTt%d" % b, bn], writes=["pb"])
                    H.op("vector", (lambda e, b=b: e.scalar_tensor_tensor(out=yv[:], in0=pb[:, 0:256], scalar=dect[:, 3:4],
                                                                         in1=ypb[b][:], op0=ALU.mult, op1=ALU.add)),
                         reads=["pb", "ypb%d" % b], writes=["yv"])
                    H.op("vector", (lambda e, b=b: e.scalar_tensor_tensor(out=Bs[:], in0=Bs[:], scalar=dect[:, 5:6],
                                                                         in1=dBb[b][:], op0=ALU.mult, op1=ALU.add)),
                         reads=["Bs", "dBb%d" % b], writes=["Bs"])
                    H.op("scalar", (lambda e, it=it: e.copy(out=B16[(it + 1) % 2][:], in_=Bs[:])), reads=["Bs"], writes=[bw])
                    H.op("vector", lambda e: e.reduce_sum(out=st8[:, 0:1], in_=yv[:], axis=AX.X), reads=["yv"], writes=["s0"])
                    H.op("vector", lambda e: e.tensor_scalar(out=st8[:, 0:1], in0=st8[:, 0:1], scalar1=1.0 / 256,
                                                             scalar2=None, op0=ALU.mult), reads=["s0"], writes=["s0"])
                    H.op("vector", lambda e: e.tensor_scalar(out=yv[:], in0=yv[:], scalar1=st8[:, 0:1], scalar2=None,
                                                             op0=ALU.subtract), reads=["yv", "s0"], writes=["yv"])
                    H.op("scalar", lambda e: e.memzero(st8[:, 1:2]), writes=["s1"])
                    H.op("scalar", lambda e: e.activation(out=junk2[:], in_=yv[:], func=AF.Square, accum_out=st8[:, 1:2]),
                         reads=["yv"], writes=["junk2", "s1"])
                    H.op("scalar", lambda e: e.activation(out=st8[:, 2:3], in_=st8[:, 1:2], func=AF.Sqrt, scale=1.0 / 256, bias=EPS),
                         reads=["s1"], writes=["s2"])
                    H.op("vector", lambda e: e.reciprocal(out=st8[:, 2:3], in_=st8[:, 2:3]), reads=["s2"], writes=["s2"])
                    H.op("vector", lambda e: e.scalar_tensor_tensor(out=yv[:], in0=yv[:], scalar=st8[:, 2:3], in1=gnrow[:],
                                                                   op0=ALU.mult, op1=ALU.mult),
                         reads=["yv", "s2", "gnrow"], writes=["yv"])
                    H.op("vector", (lambda e, b=b: e.tensor_tensor(out=ro16[:], in0=yv[:], in1=sgb[b][:], op=ALU.mult)),
                         reads=["yv", "sgb%d" % b], writes=["ro16"])
                    for j in range(2):
                        H.op("tensor", (lambda e, j=j: e.transpose(tpC[:, j * 128:(j + 1) * 128],
                                                                   ro16[:, j * 128:(j + 1) * 128], identb[:])),
                             reads=["ro16"], writes=["tpC"])
                    H.op("scalar", lambda e: e.copy(out=roT[:], in_=tpC[:, 0:256]), reads=["tpC"], writes=["roT"])
                    for j in range(2):
                        H.dma("sync", (lambda e, j=j, n=n: e.dma_start(
                            out=mixT_d[j * 128:(j + 1) * 128, n * 128:(n + 1) * 128],
                            in_=roT[:, j * 128:(j + 1) * 128])), dl[9], reads=["roT"])
                S.run_phase()
            if stop <= 2:
                return nc
        def mod_row(dst, B0, m, sem, name):
            for (r, k0, n, d0) in mod_segs(B0, 32):
                H.dma("sync", (lambda e, r=r, k0=k0, n=n, d0=d0: e.dma_start(
                    out=dst[:, d0 * 128:(d0 + n) * 128],
                    in_=bass.AP(modall_d, (r * 2 + m) * 3072 + k0 * 128, [[0, 128], [1, n * 128]]))), sem,
                    writes=[name])

        with contextlib.ExitStack() as st:
            msh = T(st, "msh", [128, 32, 1024], BF16)
            wo = [T(st, "wo%d" % i, [128, 32, 512], BF16) for i in range(2)]
            g1row = T(st, "g1row", [128, D])
            idxm = T(st, "idxm", [128, 32], I32)
            xtl = [T(st, "xtl%d" % i, [128, 512]) for i in range(2)]
            x1t = [T(st, "x1t%d" % i, [128, 512]) for i in range(2)]
            po = [P(st, "po%d" % i, [128, 512]) for i in range(2)]
            dl = pool[24:40]
            if "mixTdbg" in dbg:
                mdbg = din("mixTdbg", [512, SEQ], BF16)
                t0_ = H.dma("gpsimd", lambda e: e.dma_start(out=mixT_d.ap(), in_=mdbg.ap()), dl[0])
            else:
                t0_ = None
            tag = collective("AllGather", mixT_d, mixTall_d, [t0_])
            H.commit(tag, [], ["mixTall"])
            H.dma("sync", lambda e: e.dma_start(out=idxm[:], in_=idxm_d.ap()), dl[1], writes=["idxm"])
            mod_row(g1row, 64, 0, dl[2], "g1row")
            mview = mixTall_d.ap().rearrange("f (c t) -> (f c) t", c=NCORES)
            for j in range(32):
                H.dma("gpsimd", (lambda e, j=j: e.indirect_dma_start(
                    out=msh[:, j, :], out_offset=None, in_=mview,
                    in_offset=bass.IndirectOffsetOnAxis(ap=idxm[:, j:j + 1], axis=0))), dl[3],
                    reads=["idxm", "mixTall"], writes=["msh_%d" % j])
            wview = woutall_d.ap().rearrange("(r p) (fb n) -> r p fb n", p=128, fb=4)
            for ct in range(8):
                wb = ct % 2
                for r in range(8):
                    H.dma("gpsimd", (lambda e, r=r, ct=ct, wb=wb: e.dma_start(
                        out=wo[wb][:, r * 4:(r + 1) * 4, :], in_=wview[r, :, :, ct * 512:(ct + 1) * 512])),
                        dl[4 + wb], writes=["wo%d" % wb] if r == 0 else [], extra=[tok_wout[0]])
                H.commit((dl[4 + wb].h, dl[4 + wb].count), [], ["wo%d" % wb])
                for tb in range(8):
                    pb_ = (ct * 8 + tb) % 2
                    H.dma("sync", (lambda e, tb=tb, ct=ct, pb_=pb_: e.dma_start(
                        out=xtl[pb_][:], in_=xsh_d[tb * 128:(tb + 1) * 128, ct * 512:(ct + 1) * 512])),
                        dl[6 + pb_], writes=["xtl%d" % pb_])
                    for kc in range(32):
                        H.op("tensor", (lambda e, kc=kc, tb=tb, wb=wb, pb_=pb_: e.matmul(
                            po[pb_][:], msh[:, kc, tb * 128:(tb + 1) * 128], wo[wb][:, kc, :],
                            start=(kc == 0), stop=(kc == 31))), reads=["msh_%d" % kc, "wo%d" % wb], writes=["po%d" % pb_])
                    H.op("vector", (lambda e, ct=ct, pb_=pb_: e.tensor_tensor(
                        out=x1t[pb_][:], in0=po[pb_][:], in1=g1row[:, ct * 512:(ct + 1) * 512], op=ALU.mult)),
                        reads=["po%d" % pb_, "g1row"], writes=["x1t%d" % pb_])
                    H.op("vector", (lambda e, pb_=pb_: e.tensor_tensor(
                        out=x1t[pb_][:], in0=x1t[pb_][:], in1=xtl[pb_][:], op=ALU.add)),
                        reads=["x1t%d" % pb_, "xtl%d" % pb_], writes=["x1t%d" % pb_])
                    H.dma("sync", (lambda e, tb=tb, ct=ct, pb_=pb_: e.dma_start(
                        out=x1_d[tb * 128:(tb + 1) * 128, ct * 512:(ct + 1) * 512], in_=x1t[pb_][:])),
                        dl[8 + pb_], reads=["x1t%d" % pb_])
            S.run_phase()
        if stop <= 3:
            return nc

        with contextlib.ExitStack() as st:
            a2row = T(st, "a2row", [128, D])
            sh2row = T(st, "sh2row", [128, D])
            xb = [T(st, "xb%d" % i, [128, D]) for i in range(2)]
            h2f = T(st, "h2f", [128, D])
            h2b = T(st, "h2b", [128, D], BF16)
            h2T = T(st, "h2T", [128, 32, 128])
            wrt = T(st, "wrt", [128, 32, 72])
            brow = T(st, "brow", [128, 72])
            lg = T(st, "lg", [128, 72])
            sm = T(st, "sm", [128, 64])
            Rt_ = T(st, "Rt_", [128, 132])
            pt = [P(st, "pt%d" % i, [128, 512]) for i in range(2)]
            plg = P(st, "plg", [128, 512])
            dl = pool[0:12]
            mod_row(a2row, 128, 0, dl[0], "a2row")
            mod_row(sh2row, 96, 0, dl[1], "sh2row")
            H.dma("sync", lambda e: e.dma_start(out=h2f[:], in_=bass.AP(gains_d, 0, [[0, 128], [1, D]])), dl[2],
                  writes=["h2f"])
            H.dma("sync", lambda e: e.dma_start(out=wrt[:], in_=wr_d.ap()), dl[3], writes=["wrt"])
            H.dma("sync", lambda e: e.dma_start(out=brow[:], in_=bass.AP(br_d, 0, [[0, 128], [1, 72]])), dl[3],
                  writes=["brow"])
            H.commit((dl[3].h, dl[3].count), [], ["wrt", "brow"])
            H.op("vector", lambda e: e.scalar_tensor_tensor(out=a2row[:], in0=a2row[:], scalar=1.0, in1=h2f[:],
                                                           op0=ALU.add, op1=ALU.mult),
                 reads=["a2row", "h2f"], writes=["a2row"])
            H.op("vector", lambda e: e.memset(Rt_[:], 0.0), writes=["Rt_"])
            for tb in range(8):
                b = tb % 2
                H.dma("sync", (lambda e, tb=tb, b=b: e.dma_start(out=xb[b][:], in_=x1_d[tb * 128:(tb + 1) * 128, :])),
                      dl[4 + b], writes=["xb%d" % b])
                H.op("scalar", lambda e: e.memzero(sm[:, 0:1]), writes=["sm0"])
                H.op("scalar", (lambda e, b=b: e.activation(out=h2f[:], in_=xb[b][:], func=AF.Square,
                                                           accum_out=sm[:, 0:1])),
                     reads=["xb%d" % b], writes=["h2f", "sm0"])
                H.op("scalar", lambda e: e.activation(out=sm[:, 1:2], in_=sm[:, 0:1], func=AF.Sqrt, scale=1.0 / D,
                                                      bias=EPS), reads=["sm0"], writes=["sm1"])
                H.op("vector", lambda e: e.reciprocal(out=sm[:, 1:2], in_=sm[:, 1:2]), reads=["sm1"], writes=["sm1"])
                H.op("vector", (lambda e, b=b: e.scalar_tensor_tensor(out=h2f[:], in0=xb[b][:], scalar=sm[:, 1:2],
                                                                     in1=a2row[:], op0=ALU.mult, op1=ALU.mult)),
                     reads=["xb%d" % b, "sm1", "a2row"], writes=["h2f"])
                H.op("vector", lambda e: e.tensor_tensor(out=h2f[:], in0=h2f[:], in1=sh2row[:], op=ALU.add),
                     reads=["h2f", "sh2row"], writes=["h2f"])
                H.op("scalar", lambda e: e.copy(out=h2b[:], in_=h2f[:]), reads=["h2f"], writes=["h2b"])
                H.dma("sync", (lambda e, tb=tb: e.dma_start(out=h2_d[tb * 128:(tb + 1) * 128, :], in_=h2b[:])), dl[6],
                      reads=["h2b"])
                for g8 in range(8):
                    bk = g8 % 2
                    for j in range(4):
                        kc = g8 * 4 + j
                        H.op("tensor", (lambda e, kc=kc, j=j, bk=bk: e.matmul(
                            pt[bk][:, j * 128:(j + 1) * 128], h2f[:, kc * 128:(kc + 1) * 128], ident_f,
                            start=True, stop=True)), reads=["h2f"], writes=["pt%d" % bk])
                    if bk == 0:
                        H.op("vector", (lambda e, g8=g8, bk=bk: e.tensor_copy(
                            out=h2T[:, g8 * 4:(g8 + 1) * 4, :], in_=pt[bk][:].rearrange("p (a b) -> p a b", a=4))),
                            reads=["pt%d" % bk], writes=["h2T_%d" % g8])
                    else:
                        H.op("scalar", (lambda e, g8=g8, bk=bk: e.copy(
                            out=h2T[:, g8 * 4:(g8 + 1) * 4, :], in_=pt[bk][:].rearrange("p (a b) -> p a b", a=4))),
                            reads=["pt%d" % bk], writes=["h2T_%d" % g8])
                for kc in range(32):
                    H.op("tensor", (lambda e, kc=kc: e.matmul(plg[:, 0:72], h2T[:, kc, :], wrt[:, kc, :],
                                                              start=(kc == 0), stop=(kc == 31))),
                         reads=["h2T_%d" % (kc // 4), "wrt"], writes=["plg"])
                V = lambda fn, r, w: H.op("vector", fn, reads=r, writes=w)
                V(lambda e: e.tensor_tensor(out=lg[:], in0=plg[:, 0:72], in1=brow[:], op=ALU.add), ["plg", "brow"], ["lg"])
                V(lambda e: e.reduce_max(out=sm[:, 2:3], in_=lg[:, 0:8], axis=AX.X), ["lg"], ["sm2"])
                V(lambda e: e.tensor_scalar(out=sm[:, 8:16], in0=lg[:, 0:8], scalar1=sm[:, 2:3], scalar2=None,
                                            op0=ALU.is_equal), ["lg", "sm2"], ["ohg"])
                V(lambda e: e.tensor_scalar(out=sm[:, 3:4], in0=sm[:, 2:3], scalar1=-1.0, scalar2=None, op0=ALU.mult),
                  ["sm2"], ["sm3"])
                H.op("scalar", lambda e: e.activation(out=sm[:, 16:24], in_=lg[:, 0:8], func=AF.Exp, bias=sm[:, 3:4],
                                                      scale=1.0), reads=["lg", "sm3"], writes=["exg"])
                V(lambda e: e.reduce_sum(out=sm[:, 4:5], in_=sm[:, 16:24], axis=AX.X), ["exg"], ["sm4"])
                V(lambda e: e.reciprocal(out=sm[:, 4:5], in_=sm[:, 4:5]), ["sm4"], ["sm4"])
                V(lambda e: e.memset(sm[:, 24:32], 0.0), [], ["el"])
                for g in range(8):
                    V((lambda e, g=g: e.scalar_tensor_tensor(out=sm[:, 24:32], in0=lg[:, 8 + g * 8:16 + g * 8],
                                                             scalar=sm[:, 8 + g:9 + g], in1=sm[:, 24:32],
                                                             op0=ALU.mult, op1=ALU.add)), ["lg", "ohg", "el"], ["el"])
                V(lambda e: e.reduce_max(out=sm[:, 5:6], in_=sm[:, 24:32], axis=AX.X), ["el"], ["m1"])
                V(lambda e: e.tensor_scalar(out=sm[:, 32:40], in0=sm[:, 24:32], scalar1=sm[:, 5:6], scalar2=None,
                                            op0=ALU.is_equal), ["el", "m1"], ["oh1"])
                V(lambda e: e.scalar_tensor_tensor(out=sm[:, 40:48], in0=sm[:, 32:40], scalar=-1e30, in1=sm[:, 24:32],
                                                   op0=ALU.mult, op1=ALU.add), ["oh1", "el"], ["el2"])
                V(lambda e: e.reduce_max(out=sm[:, 6:7], in_=sm[:, 40:48], axis=AX.X), ["el2"], ["m2"])
                V(lambda e: e.tensor_scalar(out=sm[:, 48:56], in0=sm[:, 40:48], scalar1=sm[:, 6:7], scalar2=None,
                                            op0=ALU.is_equal), ["el2", "m2"], ["oh2"])
                V(lambda e: e.tensor_tensor(out=sm[:, 7:8], in0=sm[:, 6:7], in1=sm[:, 5:6], op=ALU.subtract),
                  ["m1", "m2"], ["dm"])
                H.op("scalar", lambda e: e.activation(out=sm[:, 7:8], in_=sm[:, 7:8], func=AF.Exp), reads=["dm"],
                     writes=["dm"])
                V(lambda e: e.tensor_scalar(out=sm[:, 7:8], in0=sm[:, 7:8], scalar1=1.0, scalar2=None, op0=ALU.add),
                  ["dm"], ["dm"])
                V(lambda e: e.reciprocal(out=sm[:, 56:57], in_=sm[:, 7:8]), ["dm"], ["pe1"])
                V(lambda e: e.tensor_scalar(out=sm[:, 57:58], in0=sm[:, 56:57], scalar1=-1.0, scalar2=1.0,
                                            op0=ALU.mult, op1=ALU.add), ["pe1"], ["pe2"])
                V(lambda e: e.tensor_scalar(out=Rt_[:, 128:130], in0=sm[:, 56:58], scalar1=sm[:, 4:5], scalar2=None,
                                            op0=ALU.mult), ["pe1", "pe2", "sm4"], ["Rt_"])
                for g in range(8):
                    V((lambda e, g=g: e.tensor_scalar(out=Rt_[:, g * 8:g * 8 + 8], in0=sm[:, 32:40],
                                                      scalar1=sm[:, 8 + g:9 + g], scalar2=None, op0=ALU.mult)),
                      ["oh1", "ohg"], ["Rt_"])
                    V((lambda e, g=g: e.tensor_scalar(out=Rt_[:, 64 + g * 8:72 + g * 8], in0=sm[:, 48:56],
                                                      scalar1=sm[:, 8 + g:9 + g], scalar2=None, op0=ALU.mult)),
                      ["oh2", "ohg"], ["Rt_"])
                H.dma("sync", (lambda e, tb=tb: e.dma_start(out=R_d[tb * 128:(tb + 1) * 128, :], in_=Rt_[:])), dl[7],
                      reads=["Rt_"])
                if Rdbg_d is not None:
                    H.dma("sync", (lambda e, tb=tb: e.dma_start(out=Rdbg_d[tb * 128:(tb + 1) * 128, :], in_=Rt_[:])),
                          dl[8], reads=["Rt_"])
            S.run_phase()
        if stop <= 4:
            return nc
        Tmy = T(gst, "Tmy", [128, 8, 16])
        idxl = T(gst, "idxl", [128, 8 * (CAP // 128)], I32)
        with contextlib.ExitStack() as st:
            Rt = T(st, "Rt", [128, 64, 132])
            Aall = T(st, "Aall", [128, 4096], BF16)
            pre = T(st, "pre", [128, 4096])
            tot = T(st, "tot", [128, 4096])
            cum = T(st, "cum", [128, 4096])
            ecap3 = T(st, "ecap3", [128, 4096])
            iot = T(st, "iot", [128, CAP])
            tmp = T(st, "tmp5", [128, 4096])
            s6 = T(st, "s6", [128, 8, 64])
            Tt = T(st, "Tt", [128, 64, 16])
            cin = T(st, "cin", [128, 64, 8])
            Cb = [T(st, "Cb%d" % i, [128, CAP], BF16) for i in range(4)]
            U16 = T(st, "U16", [128, 128], BF16)
            one16 = T(st, "one16", [128, 128], BF16)
            selt = T(st, "selt", [128, 8])
            idxt = T(st, "idxt", [128, 8], I32)
            lrow = [T(st, "lrow%d" % i, [1, CAP]) for i in range(2)]
            lrowi = [T(st, "lrowi%d" % i, [1, CAP], I32) for i in range(2)]
            ps = [P(st, "ps%d" % i, [128, 512]) for i in range(2)]
            pt2 = [P(st, "pt2_%d" % i, [128, 512]) for i in range(2)]
            NH = CAP // 512
            pl = [[P(st, "pl%d_%d" % (i, h), [128, 512]) for h in range(NH)] for i in range(2)]
            dl = pool[12:24]
            V = lambda fn, r, w: H.op("vector", fn, reads=r, writes=w)
            tg1 = collective("AllGather", R_d, Rall_d, [])
            H.commit(tg1, [], ["Rall"])
            tg2 = collective("AllGather", h2_d, h2all_d, [tg1])
            H.commit(tg2, [], ["h2all"])
            H.dma("sync", lambda e: e.dma_start(out=Rt[:], in_=Rall_d.ap().rearrange("(tb p) c -> p tb c", p=128)),
                  dl[0], reads=["Rall"], writes=["Rt"])
            H.dma("sync", lambda e: e.dma_start(out=ecap3[:], in_=cst2_d[:, 0:4096]), dl[1], writes=["ecap3"])
            H.dma("sync", lambda e: e.dma_start(out=iot[:], in_=cst2_d[:, 4096:4096 + CAP]), dl[1], writes=["iot"])
            H.dma("sync", lambda e: e.dma_start(out=selt[:], in_=sel_d.ap()), dl[1], writes=["selt"])
            H.dma("sync", lambda e: e.dma_start(out=idxt[:], in_=idxt_d.ap()), dl[1], writes=["idxt"])
            H.commit((dl[1].h, dl[1].count), [], ["ecap3", "iot", "selt", "idxt"])
            V(lambda e: e.tensor_copy(out=U16[:], in_=cst[:, 768:896]), [], ["U16"])
            V(lambda e: e.tensor_copy(out=one16[:], in_=cst[:, 896:1024]), [], ["one16"])
            A3 = Aall[:].rearrange("p (t e) -> p t e", e=64)
            V(lambda e: e.tensor_tensor(out=A3, in0=Rt[:, :, 0:64], in1=Rt[:, :, 64:128], op=ALU.add), ["Rt"], ["Aall"])
            for g in range(8):
                b = g % 2
                H.op("tensor", (lambda e, g=g, b=b: e.matmul(ps[b][:], U16[:], Aall[:, g * 512:(g + 1) * 512],
                                                             start=True, stop=True)), reads=["U16", "Aall"],
                     writes=["ps%d" % b])
                V((lambda e, g=g, b=b: e.tensor_copy(out=pre[:, g * 512:(g + 1) * 512], in_=ps[b][:])),
                  ["ps%d" % b], ["pre%d" % g])
                H.op("tensor", (lambda e, g=g, b=b: e.matmul(pt2[b][:], one16[:], Aall[:, g * 512:(g + 1) * 512],
                                                             start=True, stop=True)), reads=["one16", "Aall"],
                     writes=["pt2_%d" % b])
                H.op("scalar", (lambda e, g=g, b=b: e.copy(out=tot[:, g * 512:(g + 1) * 512], in_=pt2[b][:])),
                     reads=["pt2_%d" % b], writes=["tot%d" % g])
            V(lambda e: e.memset(cum[:, 0:64], 0.0), [], ["cum"])
            for tb in range(1, 64):
                V((lambda e, tb=tb: e.tensor_tensor(out=cum[:, tb * 64:(tb + 1) * 64], in0=cum[:, (tb - 1) * 64:tb * 64],
                                                    in1=tot[:, (tb - 1) * 64:tb * 64], op=ALU.add)),
                  ["cum", "tot%d" % ((tb - 1) // 8)], ["cum"])
            V(lambda e: e.tensor_tensor(out=pre[:], in0=pre[:], in1=cum[:], op=ALU.add),
              ["cum"] + ["pre%d" % g for g in range(8)], ["pos"])
            pos3 = pre[:].rearrange("p (t e) -> p t e", e=64)
            tmp3 = tmp[:].rearrange("p (t e) -> p t e", e=64)
            ec3 = ecap3[:].rearrange("p (t e) -> p t e", e=64)
            for j in range(2):
                Aj = Rt[:, :, j * 64:(j + 1) * 64]
                V((lambda e, Aj=Aj: e.tensor_tensor(out=tmp3, in0=Aj, in1=pos3, op=ALU.mult)), ["Rt", "pos"], ["tmp"])
                V((lambda e, j=j: e.reduce_sum(out=s6[:, j, :], in_=tmp3, axis=AX.X)), ["tmp"], ["slot%d" % j])
                V((lambda e, Aj=Aj: e.tensor_tensor(out=tmp3, in0=Aj, in1=ec3, op=ALU.mult)), ["Rt", "ecap3", "slot%d" % j],
                  ["tmp"])
                V((lambda e, j=j: e.reduce_sum(out=s6[:, 2 + j, :], in_=tmp3, axis=AX.X)), ["tmp"], ["ecap%d" % j])
                V((lambda e, j=j: e.tensor_tensor(out=s6[:, 2 + j, :], in0=s6[:, 2 + j, :], in1=s6[:, j, :], op=ALU.add)),
                  ["ecap%d" % j, "slot%d" % j], ["ecap%d" % j])
                V((lambda e, j=j: e.tensor_scalar(out=s6[:, 4 + j, :], in0=s6[:, j, :], scalar1=float(CAP), scalar2=None,
                                                  op0=ALU.is_lt)), ["slot%d" % j], ["val%d" % j])
                V((lambda e, j=j: e.tensor_tensor(out=s6[:, 4 + j, :], in0=s6[:, 4 + j, :], in1=Rt[:, :, 128 + j],
                                                  op=ALU.mult)), ["val%d" % j, "Rt"], ["val%d" % j])
                for q in range(4):
                    lo = float(q * 16 * CAP)
                    hi = float((q + 1) * 16 * CAP)
                    V((lambda e, j=j, lo=lo: e.tensor_scalar(out=s6[:, 6, :], in0=s6[:, 2 + j, :], scalar1=lo, scalar2=None,
                                                             op0=ALU.is_ge)), ["ecap%d" % j], ["m1"])
                    V((lambda e, j=j, hi=hi: e.tensor_scalar(out=s6[:, 7, :], in0=s6[:, 2 + j, :], scalar1=hi, scalar2=None,
                                                             op0=ALU.is_lt)), ["ecap%d" % j], ["m2"])
                    V(lambda e: e.tensor_tensor(out=s6[:, 7, :], in0=s6[:, 7, :], in1=s6[:, 6, :], op=ALU.mult),
                      ["m1", "m2"], ["m2"])
                    V((lambda e, j=j, q=q, lo=lo: e.scalar_tensor_tensor(out=Tt[:, :, j * 4 + q], in0=s6[:, 2 + j, :],
                                                                         scalar=-lo, in1=s6[:, 7, :], op0=ALU.add,
                                                                         op1=ALU.mult)), ["ecap%d" % j, "m2"], ["Tt"])
                    V((lambda e, j=j, q=q: e.tensor_tensor(out=Tt[:, :, 8 + j * 4 + q], in0=s6[:, 4 + j, :],
                                                           in1=s6[:, 7, :], op=ALU.mult)), ["val%d" % j, "m2"], ["Tt"])
            H.dma("sync", lambda e: e.dma_start(out=T_d.ap().rearrange("(tb p) c -> p tb c", p=128), in_=Tt[:]), dl[2],
                  reads=["Tt"], writes=["T_d"])
            for tbl in range(8):
                H.dma("gpsimd", (lambda e, tbl=tbl: e.indirect_dma_start(
                    out=Tmy[:, tbl, :], out_offset=None, in_=T_d.ap(),
                    in_offset=bass.IndirectOffsetOnAxis(ap=idxt[:, tbl:tbl + 1], axis=0))), dl[3],
                    reads=["T_d", "idxt"], writes=["Tmy%d" % tbl])
            V(lambda e: e.tensor_tensor(out=tot[:], in0=pre[:], in1=Aall[:], op=ALU.add),
              ["pos", "Aall"] + ["tot%d" % g for g in range(8)] + ["cum"], ["posA"])
            pA4 = tot[:].rearrange("p (t g e) -> p t g e", g=8, e=8)
            V(lambda e: e.memset(cin[:], 0.0), [], ["cin"])
            for g in range(8):
                V((lambda e, g=g: e.scalar_tensor_tensor(out=cin[:], in0=pA4[:, :, g, :], scalar=selt[:, g:g + 1],
                                                         in1=cin[:], op0=ALU.mult, op1=ALU.add)),
                  ["posA", "selt", "cin"], ["cin"])
            k = 0
            for ex in range(8):
                pb_ = ex % 2
                for tb in range(64):
                    cb = k % 4
                    k += 1
                    V((lambda e, cb=cb, tb=tb, ex=ex: e.tensor_scalar(out=Cb[cb][:], in0=iot[:],
                                                                      scalar1=cin[:, tb, ex:ex + 1], scalar2=None,
                                                                      op0=ALU.is_ge)), ["iot", "cin"], ["Cb%d" % cb])
                    for h in range(NH):
                        H.op("tensor", (lambda e, cb=cb, tb=tb, pb_=pb_, h=h: e.matmul(
                            pl[pb_][h][0:1, :], one16[:, 0:1], Cb[cb][:, h * 512:(h + 1) * 512],
                            start=(tb == 0), stop=(tb == 63))),
                            reads=["one16", "Cb%d" % cb], writes=["pl%d_%d" % (pb_, h)])
                for h in range(NH):
                    V((lambda e, pb_=pb_, h=h: e.tensor_scalar(out=lrow[pb_][0:1, h * 512:(h + 1) * 512],
                                                               in0=pl[pb_][h][0:1, :], scalar1=float(SEQ - 1),
                                                               scalar2=None, op0=ALU.min)),
                      ["pl%d_%d" % (pb_, h)], ["lrow%d_%d" % (pb_, h)])
                V((lambda e, pb_=pb_: e.tensor_copy(out=lrowi[pb_][:], in_=lrow[pb_][:])),
                  ["lrow%d_%d" % (pb_, h) for h in range(NH)], ["lrowi%d" % pb_])
                H.dma("sync", (lambda e, ex=ex, pb_=pb_: e.dma_start(
                    out=lst_d[ex * CAP:(ex + 1) * CAP, :].rearrange("n o -> o n"), in_=lrowi[pb_][:])), dl[4],
                    reads=["lrowi%d" % pb_], writes=["lst_d"])
            H.commit((dl[4].h, dl[4].count), [], ["lst_d"])
            H.dma("sync", lambda e: e.dma_start(out=idxl[:], in_=lst_d.ap().rearrange("(b p) o -> p (b o)", p=128),
                                                allow_slow_non_contiguous=True),
                  dl[5], reads=["lst_d"], writes=["idxl"])
            if dbg.get("dump") == "p5a":
                H.dma("sync", lambda e: e.dma_start(out=dbgo_d[:, 1024:1152], in_=Tmy[:].rearrange("p a b -> p (a b)")),
                      dl[6], reads=["Tmy%d" % t_ for t_ in range(8)])
                V(lambda e: e.tensor_copy(out=tmp[:, 0:8 * (CAP // 128)], in_=idxl[:]), ["idxl"], ["tmpd"])
                H.dma("sync", lambda e: e.dma_start(out=dbgo_d[:, 32:32 + 8 * (CAP // 128)], in_=tmp[:, 0:8 * (CAP // 128)]),
                      dl[6], reads=["tmpd"])
            S.run_phase()
        if stop <= 5:
            return nc

        with contextlib.ExitStack() as st:
            wgt = T(st, "wgt", [128, 32, 512], BF16)
            wut = T(st, "wut", [128, 32, 512], BF16)
            wdt = T(st, "wdt", [128, 4, D], BF16)
            Xg = [T(st, "Xg%d" % i, [128, D], BF16) for i in range(2)]
            XT = T(st, "XT", [128, 32, 128], BF16)
            sgl = T(st, "sgl", [128, 512])
            hid = T(st, "hid", [128, 512], BF16)
            hidT = T(st, "hidT", [128, 512], BF16)
            Yt = [T(st, "Yt%d" % i, [128, D], BF16) for i in range(2)]
            tx = [P(st, "tx%d" % i, [128, 1024], BF16) for i in range(2)]
            pg = P(st, "pg", [128, 512])
            pu = P(st, "pu", [128, 512])
            py = [P(st, "py%d" % i, [128, 512]) for i in range(2)]
            dl = pool[24:40]
            h2rows = h2all_d.ap()
            tgy = [None]
            nblk = CAP // 128
            it = 0
            for ex in range(NEX):
                for q in range(16):
                    H.dma("gpsimd", (lambda e, ex=ex, q=q: e.dma_start(
                        out=wgt[:, q * 2:(q + 1) * 2, :], in_=wg_d[ex, :, q * 2:(q + 1) * 2, :])), dl[0],
                        writes=["wgt"] if q == 0 else [])
                H.commit((dl[0].h, dl[0].count), [], ["wgt"])
                for q in range(16):
                    H.dma("gpsimd", (lambda e, ex=ex, q=q: e.dma_start(
                        out=wut[:, q * 2:(q + 1) * 2, :], in_=wu_d[ex, :, q * 2:(q + 1) * 2, :])), dl[1],
                        writes=["wut"] if q == 0 else [])
                H.commit((dl[1].h, dl[1].count), [], ["wut"])
                for q in range(16):
                    kc, c4 = divmod(q, 4)
                    H.dma("gpsimd", (lambda e, ex=ex, kc=kc, c4=c4: e.dma_start(
                        out=wdt[:, kc, c4 * 1024:(c4 + 1) * 1024],
                        in_=wd_d[ex, :, kc, c4 * 1024:(c4 + 1) * 1024])), dl[2],
                        writes=["wdt"] if q == 0 else [])
                H.commit((dl[2].h, dl[2].count), [], ["wdt"])
                for blk in range(nblk):
                    xb_ = it % 2
                    it += 1
                    col = ex * nblk + blk
                    H.dma("gpsimd", (lambda e, xb_=xb_, col=col: e.indirect_dma_start(
                        out=Xg[xb_][:], out_offset=None, in_=h2rows,
                        in_offset=bass.IndirectOffsetOnAxis(ap=idxl[:, col:col + 1], axis=0))), dl[3 + xb_],
                        reads=["h2all"], writes=["Xg%d" % xb_])
                    for g4 in range(4):
                        bk = g4 % 2
                        for j in range(8):
                            kc = g4 * 8 + j
                            H.op("tensor", (lambda e, kc=kc, j=j, bk=bk, xb_=xb_: e.transpose(
                                tx[bk][:, j * 128:(j + 1) * 128], Xg[xb_][:, kc * 128:(kc + 1) * 128], identb[:])),
                                reads=["Xg%d" % xb_], writes=["tx%d" % bk])
                        dst = XT[:, g4 * 8:(g4 + 1) * 8, :]
                        src = tx[bk][:].rearrange("p (a b) -> p a b", a=8)
                        if bk == 0:
                            H.op("vector", (lambda e, dst=dst, src=src: e.tensor_copy(out=dst, in_=src)),
                                 reads=["tx%d" % bk], writes=["XT_%d" % g4])
                        else:
                            H.op("scalar", (lambda e, dst=dst, src=src: e.copy(out=dst, in_=src)),
                                 reads=["tx%d" % bk], writes=["XT_%d" % g4])
                    for kc in range(32):
                        H.op("tensor", (lambda e, kc=kc: e.matmul(pg[:], XT[:, kc, :], wgt[:, kc, :],
                                                                  start=(kc == 0), stop=(kc == 31))),
                             reads=["XT_%d" % (kc // 8), "wgt"], writes=["pg"])
                        H.op("tensor", (lambda e, kc=kc: e.matmul(pu[:], XT[:, kc, :], wut[:, kc, :],
                                                                  start=(kc == 0), stop=(kc == 31))),
                             reads=["XT_%d" % (kc // 8), "wut"], writes=["pu"])
                    H.op("scalar", lambda e: e.activation(out=sgl[:], in_=pg[:], func=AF.Silu), reads=["pg"], writes=["sgl"])
                    H.op("vector", lambda e: e.tensor_tensor(out=hid[:], in0=pu[:], in1=sgl[:], op=ALU.mult),
                         reads=["pu", "sgl"], writes=["hid"])
                    for j in range(4):
                        H.op("tensor", (lambda e, j=j: e.transpose(tx[0][:, j * 128:(j + 1) * 128],
                                                                   hid[:, j * 128:(j + 1) * 128], identb[:])),
                             reads=["hid"], writes=["tx0"])
                    H.op("vector", lambda e: e.tensor_copy(out=hidT[:], in_=tx[0][:, 0:512]), reads=["tx0"],
                         writes=["hidT"])
                    yb = it % 2
                    for ct in range(8):
                        pb_ = ct % 2
                        for kc in range(4):
                            H.op("tensor", (lambda e, kc=kc, ct=ct, pb_=pb_: e.matmul(
                                py[pb_][:], hidT[:, kc * 128:(kc + 1) * 128], wdt[:, kc, ct * 512:(ct + 1) * 512],
                                start=(kc == 0), stop=(kc == 3))), reads=["hidT", "wdt"], writes=["py%d" % pb_])
                        if pb_ == 0:
                            H.op("vector", (lambda e, ct=ct, yb=yb, pb_=pb_: e.tensor_copy(
                                out=Yt[yb][:, ct * 512:(ct + 1) * 512], in_=py[pb_][:])),
                                reads=["py%d" % pb_], writes=["Yt%d_%d" % (yb, ct)])
                        else:
                            H.op("scalar", (lambda e, ct=ct, yb=yb, pb_=pb_: e.copy(
                                out=Yt[yb][:, ct * 512:(ct + 1) * 512], in_=py[pb_][:])),
                                reads=["py%d" % pb_], writes=["Yt%d_%d" % (yb, ct)])
                    yrow = ((ex % 2) * nblk + blk) * 128
                    H.dma("sync", (lambda e, yb=yb, ex=ex, yrow=yrow: e.dma_start(
                        out=Y_d[ex // 2][yrow:yrow + 128, :], in_=Yt[yb][:])), dl[5 + yb],
                        reads=["Yt%d_%d" % (yb, ct) for ct in range(8)])
                if ex % 2 == 1 or ex == NEX - 1:
                    q = ex // 2
                    ydeps = [(dl[5].h, dl[5].count), (dl[6].h, dl[6].count), tgy[0]]
                    tgy[0] = collective("AllGather", Y_d[q], Yall_d[q], ydeps)
            for q in range(NEX // 2 if NEX >= 2 else 1, NQ):
                tgy[0] = collective("AllGather", Y_d[q], Yall_d[q], [tgy[0]])
            H.commit(tgy[0], [], ["Yall"])
            S.run_phase()
        if stop <= 6:
            return nc

        with contextlib.ExitStack() as st:
            g2row = T(st, "g2row", [128, D])
            fgrow = T(st, "fgrow", [128, D])
            G = [T(st, "G%d" % i, [128, D], BF16) for i in range(4)]
            xq = [T(st, "xq%d" % i, [128, D]) for i in range(2)]
            mo = T(st, "mo", [128, D])
            ot = T(st, "ot", [128, D])
            rowi = T(st, "rowi", [128, 8, 8], I32)
            s5 = T(st, "s5", [128, 4])
            dl = pool[0:12]
            V = lambda fn, r, w: H.op("vector", fn, reads=r, writes=w)
            mod_row(g2row, 160, 0, dl[0], "g2row")
            H.dma("sync", lambda e: e.dma_start(out=fgrow[:], in_=bass.AP(gains_d, D, [[0, 128], [1, D]])), dl[1],
                  writes=["fgrow"])
            V(lambda e: e.tensor_copy(out=rowi[:], in_=Tmy[:, :, 0:8]), ["Tmy%d" % t_ for t_ in range(8)], ["rowi"])
            gi = 0
            for tbl in range(8):
                b = tbl % 2
                H.dma("sync", (lambda e, tbl=tbl, b=b: e.dma_start(out=xq[b][:], in_=x1_d[tbl * 128:(tbl + 1) * 128, :])),
                      dl[6 + b], writes=["xq%d" % b])
                first = True
                for j in range(2):
                    for q in range(dbg.get("nqg", NQ)):
                        gb = gi % 4
                        gi += 1
                        c = j * 4 + q
                        H.dma("gpsimd", (lambda e, tbl=tbl, gb=gb, c=c, q=q: e.indirect_dma_start(
                            out=G[gb][:], out_offset=None, in_=Yall_d[q].ap(),
                            in_offset=bass.IndirectOffsetOnAxis(ap=rowi[:, tbl, c:c + 1], axis=0))), dl[2 + gb],
                            reads=["rowi", "Yall"], writes=["G%d" % gb])
                        if first:
                            V((lambda e, tbl=tbl, gb=gb, c=c: e.tensor_scalar(
                                out=mo[:], in0=G[gb][:], scalar1=Tmy[:, tbl, 8 + c:9 + c], scalar2=None, op0=ALU.mult)),
                              ["G%d" % gb], ["mo"])
                            first = False
                        else:
                            V((lambda e, tbl=tbl, gb=gb, c=c: e.scalar_tensor_tensor(
                                out=mo[:], in0=G[gb][:], scalar=Tmy[:, tbl, 8 + c:9 + c], in1=mo[:],
                                op0=ALU.mult, op1=ALU.add)), ["G%d" % gb, "mo"], ["mo"])
                V(lambda e: e.tensor_tensor(out=mo[:], in0=mo[:], in1=g2row[:], op=ALU.mult), ["mo", "g2row"], ["mo"])
                V((lambda e, b=b: e.tensor_tensor(out=mo[:], in0=mo[:], in1=xq[b][:], op=ALU.add)), ["mo", "xq%d" % b],
                  ["mo"])
                H.op("scalar", lambda e: e.memzero(s5[:, 0:1]), writes=["s50"])
                H.op("scalar", lambda e: e.activation(out=ot[:], in_=mo[:], func=AF.Square, accum_out=s5[:, 0:1]),
                     reads=["mo"], writes=["ot", "s50"])
                H.op("scalar", lambda e: e.activation(out=s5[:, 1:2], in_=s5[:, 0:1], func=AF.Sqrt, scale=1.0 / D,
                                                      bias=EPS), reads=["s50"], writes=["s51"])
                V(lambda e: e.reciprocal(out=s5[:, 1:2], in_=s5[:, 1:2]), ["s51"], ["s51"])
                V(lambda e: e.scalar_tensor_tensor(out=ot[:], in0=mo[:], scalar=s5[:, 1:2], in1=fgrow[:], op0=ALU.mult,
                                                   op1=ALU.mult), ["mo", "s51", "fgrow"], ["ot"])
                H.dma("sync", (lambda e, tbl=tbl: e.dma_start(out=out_d[tbl * 128:(tbl + 1) * 128, :], in_=ot[:])), dl[8],
                      reads=["ot"])
            S.run_phase()
    return nc


def make_consts():
    cst = np.zeros((128, 1024), np.float32)
    i = np.arange(128, dtype=np.float32)
    cst[:, 0:128] = np.eye(128, dtype=np.float32)
    cst[:, 128] = i + 1
    cst[:, 129] = 127 - i
    cst[:, 130] = 128
    cst[:, 131] = 128 - i
    cst[:, 132] = i
    cst[:, 133] = 128
    cst[:, 134] = 255 - i
    cst[:, 135] = 127 - i
    cst[:, 136] = i
    cst[:, 137] = 128 + i
    k = i[:, None]
    q = i[None, :]
    cst[:, 256:384] = np.maximum(q - k, 0)
    cst[:, 384:512] = (q >= k)
    cst[:, 512:640] = np.maximum(k - q, 0)
    cst[:, 640:768] = (k > q)
    cst[:, 768:896] = (k < q)
    cst[:, 896:1024] = 1.0
    return cst


def make_consts2():
    c2 = np.zeros((128, 4096 + CAP), np.float32)
    E = np.arange(64)
    g_, ex_ = E // 8, E % 8
    Ep = (ex_ // 2) * 16 + g_ * 2 + ex_ % 2
    c2[:, 0:4096] = np.tile(Ep.astype(np.float32) * CAP, 64)[None, :]
    c2[:, 4096:4096 + CAP] = np.arange(CAP, dtype=np.float32)[None, :]
    return c2


def make_rope():
    n_freq = 64
    freqs = (10000.0 ** (-np.arange(n_freq, dtype=np.float64) / n_freq))
    l = np.arange(SEQ)
    pos = np.stack([l // 64, l % 64], -1).astype(np.float64)
    ang = pos[:, :, None] * freqs[None, None, :]
    ang = ang.astype(np.float32).astype(np.float64)
    cos = np.cos(ang)
    sin = np.sin(ang)
    cosE = np.zeros((SEQ, 4, 2, 64), np.float64)
    sinE = np.zeros((SEQ, 4, 2, 64), np.float64)
    for g in range(4):
        a = g % 2
        sc = 1.0 if g < 2 else 1.0 / 16
        cosE[:, g, 0, :] = cos[:, a, :] * sc
        cosE[:, g, 1, :] = cos[:, a, :] * sc
        sinE[:, g, 0, :] = -sin[:, a, :] * sc
        sinE[:, g, 1, :] = sin[:, a, :] * sc
    tab = np.concatenate([cosE.reshape(SEQ, 512), sinE.reshape(SEQ, 512)], 1).astype(np.float32)
    return np.ascontiguousarray(tab.reshape(NCH, 128, 1024))


def fm(v):
    return np.ascontiguousarray(v.reshape(32, 128).T)


def kmajor(w):
    K, N = w.shape
    return np.ascontiguousarray(w.reshape(K // 128, 128, N).transpose(1, 0, 2))


_CONST_CACHE = {}


def prep_core(inp, i, light=False):
    if "cst" not in _CONST_CACHE:
        _CONST_CACHE["cst"] = make_consts()
        _CONST_CACHE["rope"] = make_rope()
    f = np.float32
    x = inp["x"][0]
    m = {}
    m["xsh"] = np.ascontiguousarray(x[i * TSH:(i + 1) * TSH])
    m["ctx"] = inp["ctx"][0]
    cv = np.stack([inp["c"][0], inp["c_ctx"]], 0)
    m["cvecT"] = np.ascontiguousarray(cv.reshape(2, 32, 128).transpose(2, 1, 0).reshape(128, 64))
    m["wada"] = np.ascontiguousarray(inp["w_ada"][0][:, i * 3072:(i + 1) * 3072])
    m["bada"] = np.ascontiguousarray(inp["b_ada"][0][None, i * 3072:(i + 1) * 3072])
    m["g1T"] = fm(inp["norm1_g"][0])
    m["gains"] = np.stack([inp["norm2_g"][0], inp["final_g"]], 0)
    cols = np.concatenate([np.arange(o * 2048 + i * 256, o * 2048 + (i + 1) * 256) for o in range(6)])
    m["win"] = kmajor(inp["w_in"][0][:, cols])
    m["dec"] = np.array([[inp["ret_decay_f"][0][i], inp["ret_decay_b"][0][i]]], f)
    sl = slice(i * 256, (i + 1) * 256)
    m["v256"] = np.stack([inp["ret_gn_g"][0][sl], inp["sgu_ln_g"][0][sl], inp["sgu_ln_b"][0][sl]], 0)
    m["wsT"] = np.ascontiguousarray(inp["sgu_w_s"][0][i].T)
    m["bs"] = np.ascontiguousarray(inp["sgu_b_s"][0][i][:, None])
    perm = []
    for j in range(32):
        r, fb = divmod(j, 4)
        base = r * 256 + fb * 128 if fb < 2 else 2048 + r * 256 + (fb - 2) * 128
        perm.append(np.arange(base, base + 128))
    perm = np.concatenate(perm)
    m["woutsh"] = np.ascontiguousarray(
        inp["w_out"][0][perm[i * 512:(i + 1) * 512]].reshape(4, 128, D).transpose(1, 0, 2))
    m["wr"] = kmajor(np.concatenate([inp["w_router_group"][0], inp["w_router_expert"][0]], 1))
    m["br"] = np.concatenate([inp["b_router_group"][0], inp["b_router_expert"][0]])[None, :]
    if light:
        m["wg"] = np.zeros((8, 128, 32, 512), f)
        m["wu"] = np.zeros((8, 128, 32, 512), f)
        m["wd"] = np.zeros((8, 128, 4, D), f)
    else:
        es = slice(i * 8, (i + 1) * 8)
        m["wg"] = np.ascontiguousarray(inp["w_gate"][0][es].reshape(8, 32, 128, 512).transpose(0, 2, 1, 3))
        m["wu"] = np.ascontiguousarray(inp["w_up"][0][es].reshape(8, 32, 128, 512).transpose(0, 2, 1, 3))
        m["wd"] = np.ascontiguousarray(inp["w_down"][0][es].reshape(8, 4, 128, D).transpose(0, 2, 1, 3))
    m["ropesh"] = _CONST_CACHE["rope"].reshape(SEQ, 1024)[i * TSH:(i + 1) * TSH]
    m["cst"] = _CONST_CACHE["cst"]
    m["cst2"] = make_consts2()
    sel = np.zeros((128, 8), f)
    sel[:, i] = 1.0
    m["sel"] = sel
    p = np.arange(128)
    m["idxm"] = np.ascontiguousarray(((np.arange(32)[None, :] * 128 + p[:, None]) * 8 + i).astype(np.int32))
    m["idxt"] = np.ascontiguousarray((i * TSH + np.arange(8)[None, :] * 128 + p[:, None]).astype(np.int32))
    return {k: np.ascontiguousarray(v) for k, v in m.items()}


def kernel(**inputs):
    inp = {k: np.asarray(v) for k, v in inputs.items()}
    nc = build()
    in_maps = [prep_core(inp, i) for i in range(NCORES)]
    res = run_bass_kernel_spmd(nc, in_maps, core_ids=list(range(NCORES)))
    out = np.concatenate([res.results[i]["out"] for i in range(NCORES)], 0)
    return out.reshape(1, SEQ, D).astype(np.float32)
```
